# Optimizing a Trainium2 kernel written in Bass

```python
import math
import jax, jax.numpy as jnp
from jax import lax
import numpy as np

D_MODEL = 1024
BATCH = 32
SEQ = 2048
DEPTH = 1

CHUNK = 64
D_MIX = 2 * D_MODEL
SSD_WIDTH = D_MIX // 2
SSD_HEAD_DIM = 64
SSD_HEADS = SSD_WIDTH // SSD_HEAD_DIM
SSD_GROUPS = 2
SSD_STATE = 128
SSD_CONV = 4
SSD_XBC = SSD_WIDTH + 2 * SSD_GROUPS * SSD_STATE
ML_WIDTH = D_MIX - SSD_WIDTH
ML_HEADS = 8
ML_HEAD_DIM = ML_WIDTH // ML_HEADS
ML_CONV = 4
IN_COLS = (SSD_WIDTH + SSD_XBC + SSD_HEADS) + (4 * ML_WIDTH + 2 * ML_HEADS)
MOE_GROUPS = 4
EXPERTS_PER_GROUP = 8
N_EXPERTS = MOE_GROUPS * EXPERTS_PER_GROUP
TOP_K = 2
D_EXPERT = D_MODEL // 2
EPS = 1e-6
STAB_INIT = -1e30

kernel_name = "hybrid_ssd_mlstm_hiermoe_block"


def rmsnorm(x, w):
    xf = x.astype(jnp.float32)
    y = xf * lax.rsqrt(jnp.mean(xf * xf, axis=-1, keepdims=True) + EPS)
    return (y * w.astype(jnp.float32)).astype(x.dtype)


def group_rmsnorm(y, w, groups):
    b, l, c = y.shape
    yg = y.reshape(b, l, groups, c // groups)
    yg = yg * lax.rsqrt(jnp.mean(yg * yg, axis=-1, keepdims=True) + EPS)
    return yg.reshape(b, l, c) * w.astype(jnp.float32)


def causal_dwconv(x, w, b):
    k, c = w.shape
    y = lax.conv_general_dilated(x, w[:, None, :].astype(x.dtype), window_strides=(1,),
                                 padding=[(k - 1, 0)], dimension_numbers=("NWC", "WIO", "NWC"),
                                 feature_group_count=c)
    return y + b.astype(x.dtype)


def ssd_mixer(z, xbc, dt_raw, conv_w, conv_b, dt_bias, a_log, d_skip, norm_w):
    f32 = jnp.float32
    bsz, seq, _ = z.shape
    nc = seq // CHUNK
    r = SSD_HEADS // SSD_GROUPS
    xbc = jax.nn.silu(causal_dwconv(xbc, conv_w, conv_b)).astype(f32)
    xs = xbc[..., :SSD_WIDTH].reshape(bsz, seq, SSD_HEADS, SSD_HEAD_DIM)
    bm = xbc[..., SSD_WIDTH:SSD_WIDTH + SSD_GROUPS * SSD_STATE].reshape(bsz, seq, SSD_GROUPS, SSD_STATE)
    cm = xbc[..., SSD_WIDTH + SSD_GROUPS * SSD_STATE:].reshape(bsz, seq, SSD_GROUPS, SSD_STATE)
    dt = jax.nn.softplus(dt_raw.astype(f32) + dt_bias.astype(f32))
    a = -jnp.exp(a_log.astype(f32))
    xdt = (xs * dt[..., None]).reshape(bsz, nc, CHUNK, SSD_GROUPS, r, SSD_HEAD_DIM)
    xdt = jnp.moveaxis(xdt, 1, 0)
    adt = jnp.transpose((dt * a).reshape(bsz, nc, CHUNK, SSD_GROUPS, r), (1, 0, 3, 4, 2))
    bc = jnp.moveaxis(bm.reshape(bsz, nc, CHUNK, SSD_GROUPS, SSD_STATE), 1, 0)
    cc = jnp.moveaxis(cm.reshape(bsz, nc, CHUNK, SSD_GROUPS, SSD_STATE), 1, 0)
    mask = jnp.tril(jnp.ones((CHUNK, CHUNK), dtype=bool))

    def step(state, inp):
        x_c, a_c, b_c, c_c = inp
        a_cs = jnp.cumsum(a_c, axis=-1)
        seg = jnp.where(mask, a_cs[..., :, None] - a_cs[..., None, :], -jnp.inf)
        lmat = jnp.exp(seg)
        cb = jnp.einsum('blgn,bsgn->bgls', c_c, b_c)
        y = jnp.einsum('bgls,bgrls,bsgrp->blgrp', cb, lmat, x_c)
        y = y + jnp.einsum('blgn,bgrpn,bgrl->blgrp', c_c, state, jnp.exp(a_cs))
        decay = jnp.exp(a_cs[..., -1:] - a_cs)
        state = state * jnp.exp(a_cs[..., -1])[..., None, None] + \
            jnp.einsum('blgn,bgrl,blgrp->bgrpn', b_c, decay, x_c)
        return state, y

    state0 = jnp.zeros((bsz, SSD_GROUPS, r, SSD_HEAD_DIM, SSD_STATE), f32)
    _, y = lax.scan(step, state0, (xdt, adt, bc, cc))
    y = jnp.moveaxis(y, 0, 1).reshape(bsz, seq, SSD_HEADS, SSD_HEAD_DIM)
    y = y + d_skip.astype(f32)[:, None] * xs
    y = y.reshape(bsz, seq, SSD_WIDTH) * jax.nn.silu(z.astype(f32))
    return group_rmsnorm(y, norm_w, SSD_GROUPS).astype(z.dtype)


def mlstm_mixer(qk, v, o_pre, i_pre, f_pre, conv_w, conv_b, i_bias, f_bias, norm_w):
    f32 = jnp.float32
    bsz, seq, _ = v.shape
    nc = seq // CHUNK
    qk = jax.nn.silu(causal_dwconv(qk, conv_w, conv_b)).astype(f32)
    shp = (bsz, nc, CHUNK, ML_HEADS, ML_HEAD_DIM)
    q = jnp.moveaxis(qk[..., :ML_WIDTH].reshape(shp), 1, 0)
    k = jnp.moveaxis(qk[..., ML_WIDTH:].reshape(shp), 1, 0) * (ML_HEAD_DIM ** -0.5)
    vv = jnp.moveaxis(v.astype(f32).reshape(shp), 1, 0)
    ig = i_pre.astype(f32) + i_bias.astype(f32)
    lf = jax.nn.log_sigmoid(f_pre.astype(f32) + f_bias.astype(f32))
    ig = jnp.transpose(ig.reshape(bsz, nc, CHUNK, ML_HEADS), (1, 0, 3, 2))
    lf = jnp.transpose(lf.reshape(bsz, nc, CHUNK, ML_HEADS), (1, 0, 3, 2))
    mask = jnp.tril(jnp.ones((CHUNK, CHUNK), dtype=bool))

    def step(carry, inp):
        cmem, nmem, m = carry
        q_c, k_c, v_c, i_c, f_c = inp
        bcum = jnp.cumsum(f_c, axis=-1)
        dmat = jnp.where(mask, bcum[..., :, None] - bcum[..., None, :] + i_c[..., None, :], -jnp.inf)
        m_inter = bcum + m[..., None]
        m_t = jnp.maximum(m_inter, jnp.max(dmat, axis=-1))
        w = jnp.exp(dmat - m_t[..., None])
        s = jnp.einsum('blhd,bshd->bhls', q_c, k_c) * w
        inter = jnp.exp(m_inter - m_t)
        num = jnp.einsum('bhls,bshv->blhv', s, v_c) + \
            jnp.einsum('blhk,bhkv->blhv', q_c, cmem) * jnp.swapaxes(inter, 1, 2)[..., None]
        den = jnp.sum(s, axis=-1) + inter * jnp.einsum('blhk,bhk->bhl', q_c, nmem)
        den = jnp.maximum(jnp.abs(den), jnp.exp(-m_t))
        h = num / jnp.swapaxes(den, 1, 2)[..., None]
        g = bcum[..., -1]
        a = g[..., None] - bcum + i_c
        m_new = jnp.maximum(g + m, jnp.max(a, axis=-1))
        wc = jnp.exp(a - m_new[..., None])
        carry_scale = jnp.exp(g + m - m_new)
        cmem = cmem * carry_scale[..., None, None] + jnp.einsum('bhs,bshk,bshv->bhkv', wc, k_c, v_c)
        nmem = nmem * carry_scale[..., None] + jnp.einsum('bhs,bshk->bhk', wc, k_c)
        return (cmem, nmem, m_new), h

    carry0 = (jnp.zeros((bsz, ML_HEADS, ML_HEAD_DIM, ML_HEAD_DIM), f32),
              jnp.zeros((bsz, ML_HEADS, ML_HEAD_DIM), f32),
              jnp.full((bsz, ML_HEADS), STAB_INIT, f32))
    _, h = lax.scan(step, carry0, (q, k, vv, ig, lf))
    h = jnp.moveaxis(h, 0, 1).reshape(bsz, seq, ML_HEADS, ML_HEAD_DIM)
    mu = jnp.mean(h, axis=-1, keepdims=True)
    var = jnp.mean((h - mu) ** 2, axis=-1, keepdims=True)
    h = ((h - mu) * lax.rsqrt(var + EPS)).reshape(bsz, seq, ML_WIDTH) * norm_w.astype(f32)
    h = jax.nn.sigmoid(o_pre.astype(f32)) * h
    return h.astype(v.dtype)


def hier_moe(h, wg, bg, we, be, w_gate, w_up, w_down):
    bsz, seq, d = h.shape
    t = bsz * seq
    xf = h.reshape(t, d)
    x32 = xf.astype(jnp.float32)
    p_group = jax.nn.softmax(x32 @ wg.astype(jnp.float32) + bg.astype(jnp.float32), axis=-1)
    pg, g_sel = lax.top_k(p_group, 1)
    e_logits = (x32 @ we.astype(jnp.float32) + be.astype(jnp.float32)).reshape(t, MOE_GROUPS, EXPERTS_PER_GROUP)
    e_in = jnp.take_along_axis(e_logits, g_sel[:, :, None], axis=1)[:, 0]
    top_v, top_i = lax.top_k(e_in, TOP_K)
    gates = jax.nn.softmax(top_v, axis=-1) * pg
    ids = (g_sel * EXPERTS_PER_GROUP + top_i).reshape(-1).astype(jnp.int32)
    order = jnp.argsort(ids)
    tok = order // TOP_K
    xs = xf[tok]
    sizes = jnp.bincount(ids, length=N_EXPERTS).astype(jnp.int32)
    hg = lax.ragged_dot(xs, w_gate.astype(xs.dtype), sizes)
    hu = lax.ragged_dot(xs, w_up.astype(xs.dtype), sizes)
    ys = lax.ragged_dot(jax.nn.silu(hg) * hu, w_down.astype(xs.dtype), sizes)
    ys = ys * gates.reshape(-1)[order][:, None].astype(ys.dtype)
    y = jax.ops.segment_sum(ys, tok, num_segments=t)
    return y.reshape(bsz, seq, d).astype(h.dtype)


def setup_inputs(seed: int = 0) -> dict:
    key = jax.random.key(seed)
    ks = jax.random.split(key, 26)
    nrm = lambda k, s, sc: jax.random.normal(k, s, jnp.float32) * sc
    dt0 = jnp.exp(jax.random.uniform(ks[6], (DEPTH, SSD_HEADS), jnp.float32, math.log(1e-3), math.log(1e-1)))
    return {
        "x": jax.random.normal(ks[0], (BATCH, SEQ, D_MODEL), jnp.float32),
        "norm1_w": 1.0 + nrm(ks[1], (DEPTH, D_MODEL), 0.02),
        "w_in": nrm(ks[2], (DEPTH, D_MODEL, IN_COLS), D_MODEL ** -0.5),
        "ssd_conv_w": nrm(ks[3], (DEPTH, SSD_CONV, SSD_XBC), SSD_CONV ** -0.5),
        "ssd_conv_b": nrm(ks[4], (DEPTH, SSD_XBC), 0.02),
        "ssd_dt_bias": dt0 + jnp.log(-jnp.expm1(-dt0)),
        "ssd_a_log": jnp.log(jax.random.uniform(ks[5], (DEPTH, SSD_HEADS), jnp.float32, 1.0, 16.0)),
        "ssd_d": 1.0 + nrm(ks[7], (DEPTH, SSD_HEADS), 0.1),
        "ssd_norm_w": 1.0 + nrm(ks[8], (DEPTH, SSD_WIDTH), 0.02),
        "ml_conv_w": nrm(ks[9], (DEPTH, ML_CONV, 2 * ML_WIDTH), ML_CONV ** -0.5),
        "ml_conv_b": nrm(ks[10], (DEPTH, 2 * ML_WIDTH), 0.02),
        "ml_i_bias": nrm(ks[11], (DEPTH, ML_HEADS), 0.1),
        "ml_f_bias": 3.0 + 3.0 * jax.random.uniform(ks[12], (DEPTH, ML_HEADS), jnp.float32),
        "ml_norm_w": 1.0 + nrm(ks[13], (DEPTH, ML_WIDTH), 0.02),
        "w_out": nrm(ks[14], (DEPTH, D_MIX, D_MODEL), D_MIX ** -0.5),
        "norm2_w": 1.0 + nrm(ks[15], (DEPTH, D_MODEL), 0.02),
        "router_g_w": nrm(ks[16], (DEPTH, D_MODEL, MOE_GROUPS), D_MODEL ** -0.5),
        "router_g_b": nrm(ks[17], (DEPTH, MOE_GROUPS), 0.01),
        "router_e_w": nrm(ks[18], (DEPTH, D_MODEL, N_EXPERTS), D_MODEL ** -0.5),
        "router_e_b": nrm(ks[19], (DEPTH, N_EXPERTS), 0.01),
        "exp_w_gate": nrm(ks[20], (DEPTH, N_EXPERTS, D_MODEL, D_EXPERT), D_MODEL ** -0.5),
        "exp_w_up": nrm(ks[21], (DEPTH, N_EXPERTS, D_MODEL, D_EXPERT), D_MODEL ** -0.5),
        "exp_w_down": nrm(ks[22], (DEPTH, N_EXPERTS, D_EXPERT, D_MODEL), D_EXPERT ** -0.5),
        "norm_f_w": 1.0 + nrm(ks[23], (D_MODEL,), 0.02),
    }


def reference(x, norm1_w, w_in, ssd_conv_w, ssd_conv_b, ssd_dt_bias, ssd_a_log, ssd_d, ssd_norm_w,
              ml_conv_w, ml_conv_b, ml_i_bias, ml_f_bias, ml_norm_w, w_out, norm2_w,
              router_g_w, router_g_b, router_e_w, router_e_b, exp_w_gate, exp_w_up, exp_w_down, norm_f_w):
    o1 = SSD_WIDTH
    o2 = o1 + SSD_XBC
    o3 = o2 + SSD_HEADS
    o4 = o3 + 2 * ML_WIDTH
    o5 = o4 + ML_WIDTH
    o6 = o5 + ML_WIDTH
    o7 = o6 + ML_HEADS
    for layer in range(DEPTH):
        h = rmsnorm(x, norm1_w[layer])
        p = jnp.einsum('bsd,de->bse', h, w_in[layer])
        y_ssd = ssd_mixer(p[..., :o1], p[..., o1:o2], p[..., o2:o3],
                          ssd_conv_w[layer], ssd_conv_b[layer], ssd_dt_bias[layer],
                          ssd_a_log[layer], ssd_d[layer], ssd_norm_w[layer])
        y_ml = mlstm_mixer(p[..., o3:o4], p[..., o4:o5], p[..., o5:o6], p[..., o6:o7], p[..., o7:],
                           ml_conv_w[layer], ml_conv_b[layer], ml_i_bias[layer], ml_f_bias[layer],
                           ml_norm_w[layer])
        mix = jnp.concatenate([y_ssd, y_ml], axis=-1)
        x = x + jnp.einsum('bse,ed->bsd', mix, w_out[layer])
        h2 = rmsnorm(x, norm2_w[layer])
        x = x + hier_moe(h2, router_g_w[layer], router_g_b[layer], router_e_w[layer], router_e_b[layer],
                         exp_w_gate[layer], exp_w_up[layer], exp_w_down[layer])
    return rmsnorm(x, norm_f_w)
```

```python
import numpy as np
import concourse.bass as bass
import concourse.mybir as mybir
from concourse.bass_utils import run_bass_kernel_spmd

F32 = mybir.dt.float32
BF16 = mybir.dt.bfloat16
I32 = mybir.dt.int32
AF = mybir.ActivationFunctionType
ALU = mybir.AluOpType
AX = mybir.AxisListType

D = 1024
NCOLS = 6688
EPS = 1e-6
NEG = -30000.0
SL = 512
NE = 32


LOG = []


class Tok:
    __slots__ = ("w", "r", "excl")

    def __init__(self, excl=False):
        self.w = None
        self.r = []
        self.excl = excl


class Chan:
    def __init__(self, nc, name):
        self.sem = nc.alloc_semaphore(name=name)
        self.cnt = 0

    def value_for(self, seq):
        return self.sem, seq


class Eng:
    def __init__(self, nc, name, h, raw_safe=False):
        self.nc = nc
        self.name = name
        self.h = h
        self.sem = nc.alloc_semaphore(name="se_" + name)
        self.cnt = 0
        self.seq = 0
        self.last = None
        self.incs = []
        self.seen = {}
        self.raw_safe = raw_safe
        self.chans = []
        self.chan_i = 0
        self.nwaits = 0
        self.ninst = 0

    def value_for(self, seq):
        best = None
        for s, v in reversed(self.incs):
            if s >= seq:
                best = v
            else:
                break
        if best is not None:
            return self.sem, best
        assert self.last is not None and self.seq >= seq
        self.last.then_inc(self.sem, 1)
        self.cnt += 1
        LOG.append((self.name, "inc", self.seq, self.cnt))
        self.incs.append((self.seq, self.cnt))
        if len(self.incs) > 256:
            self.incs = self.incs[-128:]
        return self.sem, self.cnt

    def wait_on(self, dep):
        src, seq = dep
        if src is self and self.raw_safe:
            return
        sem, val = src.value_for(seq)
        if self.seen.get(sem, 0) >= val:
            return
        self.h.wait_ge(sem, val)
        LOG.append((self.name, "wait", str(sem), val))
        self.nwaits += 1
        self.seen[sem] = val

    def _deps(self, R, W):
        for t in R:
            if t.w is not None:
                self.wait_on(t.w)
            if t.excl:
                for d in t.r:
                    if d[0] is not self:
                        self.wait_on(d)
        for t in W:
            if t.w is not None and t.w[0] is not self:
                self.wait_on(t.w)
            for d in t.r:
                if d[0] is not self:
                    self.wait_on(d)

    def do(self, fn, R=(), W=()):
        self._deps(R, W)
        inst = fn()
        self.seq += 1
        self.ninst += 1
        self.last = inst
        LOG.append((self.name, "inst", self.seq, type(inst).__name__))
        me = (self, self.seq)
        for t in R:
            t.r.append(me)
        for t in W:
            t.w = me
            t.r = []
        return inst

    def dma(self, out, in_, R=(), W=(), indirect=None):
        ch = self.chans[self.chan_i]
        self.chan_i = (self.chan_i + 1) % len(self.chans)
        if ch.cnt and self.seen.get(ch.sem, 0) < ch.cnt:
            self.h.wait_ge(ch.sem, ch.cnt)
            self.seen[ch.sem] = ch.cnt
        self._deps(R, W)
        if indirect is None:
            inst = self.h.dma_start(out=out, in_=in_)
        else:
            inst = self.h.indirect_dma_start(out=out, in_=in_, **indirect)
        inst.then_inc(ch.sem, 16)
        ch.cnt += 16
        self.ninst += 1
        me = (ch, ch.cnt)
        for t in R:
            t.r.append(me)
        for t in W:
            t.w = me
            t.r = []
        return inst


class KB:
    def __init__(self):
        self.nc = bass.Bass("TRN2", target_bir_lowering=False)
        nc = self.nc
        self.pe = Eng(nc, "pe", nc.tensor, raw_safe=True)
        self.act = Eng(nc, "act", nc.scalar)
        self.dve = Eng(nc, "dve", nc.vector)
        self.pool = Eng(nc, "pool", nc.gpsimd)
        self.sp = Eng(nc, "sp", nc.sync)
        self.sp.chans = [Chan(nc, f"c_sp{i}") for i in range(8)]
        self.pool.chans = [Chan(nc, f"c_pl{i}") for i in range(8)]
        self.engs = [self.pe, self.act, self.dve, self.pool, self.sp]
        self._stack = []
        self._names = 0

    def sb(self, shape, dt, name=None):
        self._names += 1
        g = self.nc.sbuf_tensor(f"s{self._names}_{name or 'sb'}", list(shape), dt)
        t = g.__enter__()
        self._stack.append(g)
        return t

    def ps(self, shape, dt, name=None):
        self._names += 1
        g = self.nc.psum_tensor(f"p{self._names}_{name or 'ps'}", list(shape), dt)
        t = g.__enter__()
        self._stack.append(g)
        return t

    def barrier(self):
        LOG.append(("all", "barrier", 0, 0))
        for e in self.engs:
            for o in self.engs:
                if o is not e and o.seq > 0:
                    e.wait_on((o, o.seq))
            for o in self.engs:
                for ch in o.chans:
                    if ch.cnt:
                        e.wait_on((ch, ch.cnt))

    def mark(self):
        return len(self._stack)

    def release(self, mark):
        while len(self._stack) > mark:
            g = self._stack.pop()
            g.__exit__(None, None, None)


class _Cut(Exception):
    pass


def build(NSEQ, L, stop_after=None, cut=None):
    try:
        return _build(NSEQ, L, stop_after, cut)
    except _Cut as e:
        K = e.args[0]
        K.barrier()
        return K


def _build(NSEQ, L, stop_after=None, cut=None):
    K = KB()

    def CUT(n):
        if cut == n:
            raise _Cut(K)

    nc = K.nc
    pe, act, dve, pool, sp = K.pe, K.act, K.dve, K.pool, K.sp
    NT = NSEQ * L
    NTILE = NT // 128
    NBLK = L // 512
    NSLOT = (2 * NT) // SL + NE
    NROW = NSLOT * SL

    def din(name, shape, dt=F32):
        return nc.dram_tensor(name, list(shape), dt, kind="ExternalInput").ap()

    def dscr(name, shape, dt):
        return nc.dram_tensor(name, list(shape), dt, kind="Internal").ap()

    x_d = din("x", [NT, D])
    win_d = din("w_in", [128, 8, NCOLS])
    wout_d = din("w_out", [128, 16, D])
    cw_d = din("cw", [128, 28, 4])
    cb_d = din("cb", [128, 28])
    n1w_d = din("n1w", [128, 8])
    n2w_d = din("n2w", [128, 8])
    rep_d = din("rep", [128, 16 * 3 + 8 * 2 + 36])
    snw_d = din("snw", [128, D])
    mnw_d = din("mnw", [128, D])
    nfw_d = din("nfw", [128, D])
    wr_d = din("wr", [128, 8, 36])
    cst_d = din("cst", [128, 128 * 4 + 1024 + 64 + 128 + 24])
    wg_d = din("wg", [NE * 128, 8 * 512])
    wu_d = din("wu", [NE * 128, 8 * 512])
    wd_d = din("wd", [NE * 128, 4 * D])
    out_d = nc.dram_tensor("out", [NT, D], F32, kind="ExternalOutput").ap()
    x1_d = nc.dram_tensor("x1s", [NT, D], F32, kind=("ExternalOutput" if stop_after else "Internal")).ap()
    lg_d = nc.dram_tensor("lgs", [NT, 36], F32, kind=("ExternalOutput" if stop_after else "Internal")).ap()
    h2_d = dscr("h2s", [NT, D], BF16)
    h2s_d = dscr("h2sorted", [NROW, D], BF16)
    ys_d = dscr("yss", [NROW, D], F32)

    PS = K.ps([128, 4096], F32, "psall")
    TB = [Tok(excl=True) for _ in range(8)]

    def bank(b, n=512, p=128):
        return PS[0:p, b * 512:b * 512 + n]

    def bankbf(b):
        return PS[:, b * 512:(b + 1) * 512].bitcast(BF16)

    def ACT(out, in_, func, R, W, **kw):
        return act.do(lambda: nc.scalar.activation(out=out, in_=in_, func=func, **kw), R, W)

    def TT(e, out, a, b, op, R, W):
        return e.do(lambda: e.h.tensor_tensor(out=out, in0=a, in1=b, op=op), R, W)

    def TS(e, out, a, s1, op0, R, W, s2=None, op1=None):
        if op1 is None:
            return e.do(lambda: e.h.tensor_scalar(out=out, in0=a, scalar1=s1, scalar2=None, op0=op0), R, W)
        return e.do(lambda: e.h.tensor_scalar(out=out, in0=a, scalar1=s1, scalar2=s2, op0=op0, op1=op1), R, W)

    def STT(out, a, s, b, op0, op1, R, W):
        return dve.do(lambda: nc.vector.scalar_tensor_tensor(out=out, in0=a, scalar=s, in1=b, op0=op0, op1=op1), R, W)

    def CP(e, out, in_, R, W):
        if e is act:
            return act.do(lambda: nc.scalar.copy(out=out, in_=in_), R, W)
        return e.do(lambda: e.h.tensor_copy(out=out, in_=in_), R, W)

    def MM(out, lhsT, rhs, start, stop, R, W):
        return pe.do(lambda: nc.tensor.matmul(out, lhsT, rhs, start=start, stop=stop), R, W)

    def TR(out, in_, ident, R, W):
        return pe.do(lambda: nc.tensor.transpose(out, in_, ident), R, W)

    def RED(out, in_, op, R, W, axis=AX.X):
        return dve.do(lambda: nc.vector.tensor_reduce(out=out, in_=in_, axis=axis, op=op), R, W)

    def bc(ap, shape):
        return ap.broadcast_to(list(shape))

    cst = K.sb([128, 128 * 4 + 1024 + 64 + 128 + 24], F32, "cst")
    t_c = Tok()
    sp.dma(cst[:], cst_d, W=[t_c])
    identf = cst[:, 0:128]
    trif = cst[:, 128:256]
    onesf = cst[:, 256:384]
    ustr = cst[:, 384:512]
    sel8 = cst[0:8, 512:1536]
    thr = cst[:, 1536:1600]
    slotid = cst[:, 1600:1728]
    rowoff = cst[:, 1728:1752]
    identb = K.sb([128, 128], BF16, "identb")
    onesb = K.sb([128, 128], BF16, "onesb")
    ustrb = K.sb([128, 128], BF16, "ustrb")
    maskb = K.sb([128, 8, 128], BF16, "maskb")
    t_cb = Tok()
    CP(dve, identb[:], identf, [t_c], [t_cb])
    CP(dve, onesb[:], onesf, [t_c], [t_cb])
    CP(dve, ustrb[:], ustr, [t_c], [t_cb])
    for h in range(8):
        TS(dve, maskb[:, h, :], trif, -1.0, ALU.add, [t_c], [t_cb], s2=-NEG, op1=ALU.mult)

    rep = K.sb([128, 100], F32, "rep")
    t_rep = Tok()
    sp.dma(rep[:], rep_d, W=[t_rep])
    dtb_b = rep[:, 0:16]
    alog_b = rep[:, 16:32]
    dsk_b = rep[:, 32:48]
    ib_b = rep[:, 48:56]
    fb_b = rep[:, 56:64]
    rb_b = rep[:, 64:100]
    a_b = K.sb([128, 16], F32, "a_b")
    t_ab = Tok()
    ACT(a_b[:], alog_b, AF.Exp, [t_rep], [t_ab])
    TS(dve, a_b[:], a_b[:], -1.0, ALU.mult, [t_ab], [t_ab])
    n1w = K.sb([128, 8], F32, "n1w")
    n2w = K.sb([128, 8], F32, "n2w")
    cw = K.sb([128, 28, 4], F32, "cw")
    cbias = K.sb([128, 28], F32, "cbias")
    t_par = Tok()
    sp.dma(n1w[:], n1w_d, W=[t_par])
    sp.dma(n2w[:], n2w_d, W=[t_par])
    sp.dma(cw[:], cw_d, W=[t_par])
    sp.dma(cbias[:], cb_d, W=[t_par])
    wr = K.sb([128, 8, 36], F32, "wr")
    sp.dma(wr[:], wr_d, W=[t_par])
    wsmf = K.sb([128, 8, 32], F32, "wsmf")
    wsm = K.sb([128, 8, 32], BF16, "wsm")
    t_wsm = Tok()
    sp.dma(wsmf[:], win_d[:, :, 2560:2592], W=[t_wsm])
    CP(dve, wsm[:], wsmf[:], [t_wsm], [t_wsm])

    zt = K.sb([128, D], BF16, "zt")
    t_zt = Tok()
    pool.do(lambda: nc.gpsimd.memset(zt[:], 0.0), W=[t_zt])
    zview = h2s_d.rearrange("(n p) d -> n p d", p=128)
    t_zero = []
    for n in range(NROW // 128):
        tz = Tok()
        sp.dma(zview[n], zt[:], R=[t_zt], W=[tz])
        t_zero.append(tz)
    CUT(0)
    ph1 = K.mark()
    snw = K.sb([128, D], BF16, "snw")
    mnw = K.sb([128, D], BF16, "mnw")
    t_nw = Tok()
    pool.dma(snw[:], snw_d, W=[t_nw])
    pool.dma(mnw[:], mnw_d, W=[t_nw])
    dgp = [K.sb([128, 4, 4, 128], BF16, f"dgp{i}") for i in range(2)]
    t_dgp = [Tok() for _ in range(2)]
    t_dg = Tok()
    dpar = [0]
    dgD = K.sb([128, 16, 128], BF16, "dgD")
    for h in range(16):
        TS(dve, dgD[:, h, :], identf, dsk_b[:, h:h + 1], ALU.mult, [t_c, t_rep], [t_dg])
    wout = K.sb([128, 16, D], BF16, "wout")
    t_wout = Tok()
    for e2 in range(4):
        pool.dma(wout[:, e2 * 4:(e2 + 1) * 4, :], wout_d[:, e2 * 4:(e2 + 1) * 4, :], W=[t_wout])

    NWB = 2
    wbuf = [K.sb([128, 8, 512], BF16, f"wbuf{i}") for i in range(NWB)]
    t_wbuf = [Tok() for _ in range(NWB)]
    xt = K.sb([128, D], F32, "xt")
    t_xt = Tok()
    junk = K.sb([128, D], BF16, "junk")
    t_junk = Tok()
    xn = K.sb([128, D], BF16, "xn")
    t_xn = Tok()
    st4 = K.sb([128, 16], F32, "st4")
    t_st4 = Tok()
    hnT = K.sb([128, 8, 512], BF16, "hnT")
    t_hnT = Tok()
    pb = [K.sb([128, 515], BF16, f"pb{i}") for i in range(2)]
    t_pb = [Tok() for _ in range(2)]
    hal = K.sb([128, 28, 4], BF16, "hal")
    t_hal = [Tok() for _ in range(28)]
    cvS = K.sb([128, 12, 512], BF16, "cvS")
    t_cvS = Tok()
    cvM = K.sb([128, 16, 512], BF16, "cvM")
    t_cvM = Tok()
    zs = K.sb([128, 4, D], BF16, "zs")
    t_zs = Tok()
    vtm = K.sb([128, 4, D], BF16, "vtm")
    t_v = Tok()
    so = K.sb([128, 4, D], BF16, "so")
    t_so = Tok()
    gates = K.sb([128, 4, 32], F32, "gates")
    t_g = Tok()
    gs = K.sb([128, 4, 16 * 2 + 8 * 2], F32, "gs")
    t_gs = Tok()
    sm = K.sb([128, 160], F32, "sm")
    t_sm = Tok()
    csT = K.sb([8, 4, 128], F32, "csT")
    t_csT = Tok()
    dve.do(lambda: nc.vector.memset(csT[:, 3, :], -1.0), W=[t_csT])
    bd = K.sb([8, 8, 128], F32, "bd")
    t_bd = Tok()
    Lt = K.sb([128, 8, 128], BF16, "Lt")
    t_Lt = Tok()
    Mt = K.sb([128, 8, 128], BF16, "Mt")
    t_Mt = Tok()
    cbt = K.sb([128, 128], BF16, "cbt")
    t_cbt = Tok()
    xs_tm = K.sb([128, D], BF16, "xs_tm")
    t_xs = Tok()
    xdt_tm = K.sb([128, D], BF16, "xdt_tm")
    t_xdt = Tok()
    xdec = K.sb([128, D], BF16, "xdec")
    t_xdec = Tok()
    B_tm = K.sb([128, 256], BF16, "B_tm")
    t_Btm = Tok()
    k_tm = K.sb([128, D], BF16, "k_tm")
    t_ktm = Tok()
    kk = K.sb([128, D], BF16, "kk")
    t_kk = Tok()
    fa = K.sb([128, D], F32, "fa")
    t_fa = Tok()
    fb = K.sb([128, D], F32, "fb")
    t_fb = Tok()
    ynb = K.sb([128, D], BF16, "ynb")
    t_ynb = Tok()
    mixT = K.sb([128, 16, 128], BF16, "mixT")
    t_mixT = Tok()
    stS = K.sb([128, D], F32, "stS")
    t_stS = Tok()
    stSb = K.sb([128, D], BF16, "stSb")
    t_stSb = Tok()
    Cm = K.sb([128, D], F32, "Cm")
    t_Cm = Tok()
    Cmb = K.sb([128, D], BF16, "Cmb")
    t_Cmb = Tok()
    nm = K.sb([128, 8], F32, "nm")
    t_nm = Tok()
    nmb = K.sb([128, 8], BF16, "nmb")
    t_nmb = Tok()
    xhb = K.sb([128, D], BF16, "xhb")
    t_xhb = Tok()
    h2T = K.sb([128, 8, 128], F32, "h2T")
    t_h2T = Tok()
    lgt = K.sb([128, 36], F32, "lgt")
    t_lgt = Tok()

    pieces = []
    for i in range(3):
        pieces.append(("feat", i * 512, 512, ("S", i * 4)))
    for i in range(2):
        pieces.append(("tok", 1536 + i * 512, 512, ("z", i)))
    pieces.append(("gate", 2560, 32, ("g", 0)))
    for i in range(4):
        pieces.append(("feat", 2592 + i * 512, 512, ("M", i * 4)))
    for i in range(2):
        pieces.append(("tok", 4640 + i * 512, 512, ("v", i)))
    for i in range(2):
        pieces.append(("tok", 5664 + i * 512, 512, ("o", i)))
    NP = len(pieces)
    wq = []
    piece_ctr = [0]

    def issue_piece_load(gi):
        kind, c0, ncl, arg = pieces[gi % NP]
        slot = gi % NWB
        if kind == "gate":
            return
        pool.dma(wbuf[slot][:, :, 0:ncl], win_d[:, :, c0:c0 + ncl], W=[t_wbuf[slot]])

    total_pieces = NSEQ * NBLK * NP
    nxt_load = [0]

    def ensure_loaded(gi):
        while nxt_load[0] < min(total_pieces, gi + NWB):
            issue_piece_load(nxt_load[0])
            nxt_load[0] += 1

    ppar = [0]
    cpar = [0]
    cvpar = [0]

    for seq in range(NSEQ):
        dve.do(lambda: nc.vector.memset(stS[:], 0.0), W=[t_stS])
        dve.do(lambda: nc.vector.memset(stSb[:], 0.0), W=[t_stSb])
        dve.do(lambda: nc.vector.memset(Cm[:], 0.0), W=[t_Cm])
        dve.do(lambda: nc.vector.memset(Cmb[:], 0.0), W=[t_Cmb])
        dve.do(lambda: nc.vector.memset(nm[:], 0.0), W=[t_nm])
        dve.do(lambda: nc.vector.memset(nmb[:], 0.0), W=[t_nmb])
        for ci in range(28):
            pool.do(lambda ci=ci: nc.gpsimd.memset(hal[:, ci, :], 0.0), W=[t_hal[ci]])
        for blk in range(NBLK):
            tok0 = seq * L + blk * 512
            gbase = (seq * NBLK + blk) * NP
            ensure_loaded(gbase)
            for t in range(4):
                sp.dma(xt[:], x_d[tok0 + t * 128: tok0 + (t + 1) * 128, :], W=[t_xt])
                ACT(junk[:], xt[:], AF.Square, [t_xt], [t_junk, t_st4], accum_out=st4[:, 0:1])
                ACT(st4[:, 1:2], st4[:, 0:1], AF.Ln, [t_st4], [t_st4], scale=1.0 / D, bias=EPS)
                ACT(st4[:, 2:3], st4[:, 1:2], AF.Exp, [t_st4], [t_st4], scale=-0.5)
                TS(dve, xn[:], xt[:], st4[:, 2:3], ALU.mult, [t_xt, t_st4], [t_xn])
                b0 = 0 if t % 2 == 0 else 7
                for kc in range(8):
                    TR(bankbf(b0)[:, kc * 128:(kc + 1) * 128], xn[:, kc * 128:(kc + 1) * 128], identb[:],
                       [t_xn, t_cb], [TB[b0]])
                TT(dve, hnT[:, :, t * 128:(t + 1) * 128],
                   bankbf(b0).rearrange("p (k c) -> p k c", k=8),
                   bc(n1w[:].unsqueeze(2), [128, 8, 128]), ALU.mult, [TB[b0], t_par], [t_hnT])
            CUT(1)
            for pi in range(NP):
                gi = gbase + pi
                ensure_loaded(gi)
                kind, c0, ncl, arg = pieces[pi]
                slot = gi % NWB
                wb = wbuf[slot]
                tw = t_wbuf[slot]
                if kind == "gate":
                    wb = wsm
                    tw = t_wsm
                    kind = "tok"
                if kind == "feat":
                    which, ci0 = arg
                    dq = dpar[0]
                    dpar[0] ^= 1
                    for cc in range(4):
                        gci_ = (ci0 + cc) if which == "S" else 12 + ci0 + cc
                        for k in range(4):
                            TS(dve, dgp[dq][:, cc, k, :], identf, cw[:, gci_, k:k + 1], ALU.mult, [t_c, t_par], [t_dgp[dq]])
                    IPB = [1, 2, 4, 5]
                    CVB = [3, 6]

                    def inproj(cc):
                        ci = ci0 + cc
                        gci = ci if which == "S" else 12 + ci
                        b = IPB[ppar[0] % 4]
                        ppar[0] += 1
                        for kc in range(8):
                            MM(bank(b), wb[:, kc, cc * 128:(cc + 1) * 128], hnT[:, kc, :], kc == 0, kc == 7,
                               [tw, t_hnT], [TB[b]])
                        q = cpar[0] % 2
                        cpar[0] += 1
                        CP(pool, pb[q][:, 0:3], hal[:, gci, 0:3], [t_hal[gci]], [t_pb[q]])
                        CP(act, pb[q][:, 3:515], bank(b), [TB[b]], [t_pb[q]])
                        CP(pool, hal[:, gci, 0:3], pb[q][:, 512:515], [t_pb[q]], [t_hal[gci]])
                        return q

                    def conv(cc, q):
                        ci = ci0 + cc
                        gci = ci if which == "S" else 12 + ci
                        cb_ = CVB[cvpar[0] % 2]
                        cvpar[0] += 1
                        for k in range(4):
                            MM(bank(cb_), dgp[dq][:, cc, k, :], pb[q][:, k:k + 512], k == 0, k == 3,
                               [t_dgp[dq], t_pb[q]], [TB[cb_]])
                        if which == "S":
                            ACT(cvS[:, ci, :], bank(cb_), AF.Silu, [TB[cb_], t_par], [t_cvS], bias=cbias[:, gci:gci + 1])
                        else:
                            ACT(cvM[:, ci, :], bank(cb_), AF.Silu, [TB[cb_], t_par], [t_cvM], bias=cbias[:, gci:gci + 1])

                    qprev = inproj(0)
                    for cc in range(1, 4):
                        qn = inproj(cc)
                        conv(cc - 1, qprev)
                        qprev = qn
                    conv(3, qprev)
                else:
                    which, half = arg
                    for t in range(4):
                        b = [1, 2, 4, 5][ppar[0] % 4]
                        ppar[0] += 1
                        for kc in range(8):
                            MM(bank(b, ncl), hnT[:, kc, t * 128:(t + 1) * 128], wb[:, kc, 0:ncl], kc == 0, kc == 7,
                               [tw, t_hnT], [TB[b]])
                        if which == "z":
                            ACT(zs[:, t, half * 512:(half + 1) * 512], bank(b), AF.Silu, [TB[b]], [t_zs])
                        elif which == "v":
                            CP(act, vtm[:, t, half * 512:(half + 1) * 512], bank(b), [TB[b]], [t_v])
                        elif which == "o":
                            ACT(so[:, t, half * 512:(half + 1) * 512], bank(b), AF.Sigmoid, [TB[b]], [t_so])
                        else:
                            CP(dve, gates[:, t, :], bank(b, 32), [TB[b]], [t_g])
                CUT(10 + pi)
            CUT(2)
            dtv = gs[:, :, 0:16]
            adt = gs[:, :, 16:32]
            spv = gs[:, :, 32:40]
            ipv = gs[:, :, 40:48]
            TT(dve, dtv, gates[:, :, 0:16], bc(dtb_b.unsqueeze(1), [128, 4, 16]), ALU.add, [t_g, t_rep], [t_gs])
            ACT(dtv, dtv, AF.Exp, [t_gs], [t_gs])
            ACT(dtv, dtv, AF.Ln, [t_gs], [t_gs], bias=1.0)
            TT(dve, adt, dtv, bc(a_b[:].unsqueeze(1), [128, 4, 16]), ALU.mult, [t_gs, t_ab], [t_gs])
            TT(dve, spv, gates[:, :, 24:32], bc(fb_b.unsqueeze(1), [128, 4, 8]), ALU.add, [t_g, t_rep], [t_gs])
            ACT(spv, spv, AF.Exp, [t_gs], [t_gs], scale=-1.0)
            ACT(spv, spv, AF.Ln, [t_gs], [t_gs], bias=1.0)
            TT(dve, ipv, gates[:, :, 16:24], bc(ib_b.unsqueeze(1), [128, 4, 8]), ALU.add, [t_g, t_rep], [t_gs])
            TS(dve, ipv, ipv, float(np.log(128.0 ** -0.5)), ALU.add, [t_gs], [t_gs])

            CUT(3)
            import os as _os
            for t in range(int(_os.environ.get("KT0", "0")), 4):
                tc_ = slice(t * 128, (t + 1) * 128)
                for cc in range(8):
                    TR(bankbf(0)[:, cc * 128:(cc + 1) * 128], cvS[:, cc, tc_], identb[:], [t_cvS, t_cb], [TB[0]])
                CUT(200 + 10 * t + 0)
                CP(act, xs_tm[:], bankbf(0), [TB[0]], [t_xs])
                CUT(200 + 10 * t + 7)
                TT(dve, xdt_tm[:].rearrange("p (h c) -> p h c", h=16), xs_tm[:].rearrange("p (h c) -> p h c", h=16),
                   bc(gs[:, t, 0:16].unsqueeze(2), [128, 16, 64]), ALU.mult, [t_xs, t_gs], [t_xdt])
                for g in range(2):
                    TR(bankbf(0)[:, g * 128:(g + 1) * 128], cvS[:, 8 + g, tc_], identb[:], [t_cvS, t_cb], [TB[0]])
                CP(act, B_tm[:], bankbf(0)[:, 0:256], [TB[0]], [t_Btm])
                CUT(200 + 10 * t + 1)
                smp = bank(3)
                MM(smp[:, 0:16], trif, gs[:, t, 16:32], True, True, [t_c, t_gs], [TB[3]])
                MM(smp[:, 16:32], onesf, gs[:, t, 16:32], True, True, [t_c, t_gs], [TB[3]])
                for g in range(2):
                    MM(smp[0:8, 32 + g * 128: 160 + g * 128], gs[:, t, 16 + g * 8:24 + g * 8], trif, True, True,
                       [t_c, t_gs], [TB[3]])
                CP(dve, sm[:, 0:32], smp[:, 0:32], [TB[3]], [t_sm])
                TS(dve, csT[:, 0:2, :].rearrange("p a b -> p (a b)"), smp[0:8, 32:288], -1.0, ALU.mult, [TB[3]], [t_csT])
                ACT(sm[:, 32:48], sm[:, 0:16], AF.Exp, [t_sm], [t_sm])
                TT(dve, sm[:, 48:64], sm[:, 16:32], sm[:, 0:16], ALU.subtract, [t_sm], [t_sm])
                ACT(sm[:, 48:64], sm[:, 48:64], AF.Exp, [t_sm], [t_sm])
                ACT(sm[:, 64:80], sm[:, 16:32], AF.Exp, [t_sm], [t_sm])
                CUT(200 + 10 * t + 2)
                for g in range(2):
                    MM(bank(6 + g), cvS[:, 10 + g, tc_], stSb[:, g * 512:(g + 1) * 512], True, True,
                       [t_cvS, t_stSb], [TB[6 + g]])
                CUT(200 + 10 * t + 3)
                for g in range(2):
                    TT(dve, bd[:], sel8.rearrange("p (h l) -> p h l", h=8),
                       bc(csT[:, g, :].unsqueeze(1), [8, 8, 128]), ALU.mult, [t_c, t_csT], [t_bd])
                    for hb in range(2):
                        eb_ = bank(4 + hb)
                        cs_ = slice(hb * 512, (hb + 1) * 512)
                        MM(eb_, csT[:, g, :], sel8[:, cs_], True, False, [t_csT, t_c], [TB[4 + hb]])
                        MM(eb_, csT[:, 3, :], bd[:].rearrange("p h l -> p (h l)")[:, cs_], False, False,
                           [t_bd, t_csT], [TB[4 + hb]])
                        MM(eb_, identb[:], maskb[:, hb * 4:(hb + 1) * 4, :].rearrange("p h l -> p (h l)"), False, True,
                           [t_cb], [TB[4 + hb]])
                    ACT(Lt[:].rearrange("p h l -> p (h l)"), PS[:, 4 * 512:6 * 512], AF.Exp, [TB[4], TB[5]], [t_Lt])
                    if g == 1:
                        CUT(200 + 10 * t + 4)
                    MM(bank(3, 128), cvS[:, 8 + g, tc_], cvS[:, 10 + g, tc_], True, True, [t_cvS], [TB[3]])
                    CP(act, cbt[:], bank(3, 128), [TB[3]], [t_cbt])
                    TT(dve, Mt[:], Lt[:], bc(cbt[:].unsqueeze(1), [128, 8, 128]), ALU.mult, [t_Lt, t_cbt], [t_Mt])
                    for hh in range(8):
                        h = g * 8 + hh
                        ob = bank(1 + g)[:, hh * 64:(hh + 1) * 64]
                        MM(ob, Mt[:, hh, :], xdt_tm[:, h * 64:(h + 1) * 64], True, False, [t_Mt, t_xdt], [TB[1 + g]])
                        MM(ob, dgD[:, h, :], xs_tm[:, h * 64:(h + 1) * 64], False, True, [t_dg, t_xs], [TB[1 + g]])
                for hh in range(8):
                    TR(bankbf(0)[:, hh * 128:(hh + 1) * 128], cvM[:, 8 + hh, tc_], identb[:], [t_cvM, t_cb], [TB[0]])
                CP(act, k_tm[:], bankbf(0), [TB[0]], [t_ktm])
                smp = bank(3)
                MM(smp[:, 0:8], trif, gs[:, t, 32:40], True, True, [t_c, t_gs], [TB[3]])
                MM(smp[:, 8:16], onesf, gs[:, t, 32:40], True, True, [t_c, t_gs], [TB[3]])
                MM(smp[0:8, 32:160], gs[:, t, 40:48], identf, True, False, [t_c, t_gs], [TB[3]])
                MM(smp[0:8, 32:160], gs[:, t, 32:40], trif, False, True, [t_c, t_gs], [TB[3]])
                MM(smp[0:8, 160:288], gs[:, t, 32:40], trif, True, True, [t_c, t_gs], [TB[3]])
                CP(dve, sm[:, 80:96], smp[:, 0:16], [TB[3]], [t_sm])
                CP(dve, csT[:, 2, :], smp[0:8, 32:160], [TB[3]], [t_csT])
                ACT(sm[:, 96:104], sm[:, 80:88], AF.Exp, [t_sm], [t_sm], scale=-1.0)
                TT(dve, sm[:, 104:112], sm[:, 80:88], sm[:, 88:96], ALU.subtract, [t_sm], [t_sm])
                TT(dve, sm[:, 104:112], sm[:, 104:112], gs[:, t, 40:48], ALU.add, [t_sm, t_gs], [t_sm])
                ACT(sm[:, 104:112], sm[:, 104:112], AF.Exp, [t_sm], [t_sm])
                ACT(sm[:, 112:120], sm[:, 88:96], AF.Exp, [t_sm], [t_sm], scale=-1.0)
                TT(dve, bd[:], sel8.rearrange("p (h l) -> p h l", h=8),
                   bc(smp[0:8, 160:288].unsqueeze(1), [8, 8, 128]), ALU.mult, [t_c, TB[3]], [t_bd])
                for hb in range(2):
                    eb_ = bank(4 + hb)
                    cs_ = slice(hb * 512, (hb + 1) * 512)
                    MM(eb_, csT[:, 2, :], sel8[:, cs_], True, False, [t_csT, t_c], [TB[4 + hb]])
                    MM(eb_, csT[:, 3, :], bd[:].rearrange("p h l -> p (h l)")[:, cs_], False, False,
                       [t_bd, t_csT], [TB[4 + hb]])
                    MM(eb_, identb[:], maskb[:, hb * 4:(hb + 1) * 4, :].rearrange("p h l -> p (h l)"), False, True,
                       [t_cb], [TB[4 + hb]])
                ACT(Lt[:].rearrange("p h l -> p (h l)"), PS[:, 4 * 512:6 * 512], AF.Exp, [TB[4], TB[5]], [t_Lt])
                CUT(200 + 10 * t + 5)
                TT(dve, fa[:].rearrange("p (h c) -> p h c", h=16), PS[:, 6 * 512:8 * 512].rearrange("p (h c) -> p h c", h=16),
                   bc(sm[:, 32:48].unsqueeze(2), [128, 16, 64]), ALU.mult, [TB[6], TB[7], t_sm], [t_fa])
                TT(dve, fa[:], PS[:, 1 * 512:3 * 512], fa[:], ALU.add, [TB[1], TB[2], t_fa], [t_fa])
                TT(dve, fa[:], fa[:], zs[:, t, :], ALU.mult, [t_fa, t_zs], [t_fa])
                for g in range(2):
                    ACT(junk[:, g * 512:(g + 1) * 512], fa[:, g * 512:(g + 1) * 512], AF.Square, [t_fa], [t_junk, t_st4],
                        accum_out=st4[:, 4 + g:5 + g])
                ACT(st4[:, 6:8], st4[:, 4:6], AF.Ln, [t_st4], [t_st4], scale=1.0 / 512, bias=EPS)
                ACT(st4[:, 8:10], st4[:, 6:8], AF.Exp, [t_st4], [t_st4], scale=-0.5)
                for g in range(2):
                    STT(ynb[:, g * 512:(g + 1) * 512], fa[:, g * 512:(g + 1) * 512], st4[:, 8 + g:9 + g],
                        snw[:, g * 512:(g + 1) * 512], ALU.mult, ALU.mult, [t_fa, t_st4, t_nw], [t_ynb])
                for cc in range(8):
                    TR(bankbf(0)[:, cc * 128:(cc + 1) * 128], ynb[:, cc * 128:(cc + 1) * 128], identb[:], [t_ynb, t_cb], [TB[0]])
                CP(act, mixT[:, 0:8, :].rearrange("p a b -> p (a b)"), bankbf(0), [TB[0]], [t_mixT])
                CUT(200 + 10 * t + 6)
                TT(dve, xdec[:].rearrange("p (h c) -> p h c", h=16), xdt_tm[:].rearrange("p (h c) -> p h c", h=16),
                   bc(sm[:, 48:64].unsqueeze(2), [128, 16, 64]), ALU.mult, [t_xdt, t_sm], [t_xdec])
                for g in range(2):
                    MM(bank(6 + g), B_tm[:, g * 128:(g + 1) * 128], xdec[:, g * 512:(g + 1) * 512], True, True,
                       [t_Btm, t_xdec], [TB[6 + g]])
                TT(dve, stS[:].rearrange("p (h c) -> p h c", h=16), stS[:].rearrange("p (h c) -> p h c", h=16),
                   bc(sm[:, 64:80].unsqueeze(2), [128, 16, 64]), ALU.mult, [t_stS, t_sm], [t_stS])
                TT(dve, stS[:], stS[:], PS[:, 6 * 512:8 * 512], ALU.add, [t_stS, TB[6], TB[7]], [t_stS])
                CP(act, stSb[:], stS[:], [t_stS], [t_stSb])

                CUT(4)
                CUT(100 + 10 * t + 4)
                if 'ml' not in _os.environ.get('KSKIP', ''):
                    for hh in range(8):
                        b = 6 + hh // 4
                        MM(bank(b)[:, (hh % 4) * 128:(hh % 4 + 1) * 128], cvM[:, 8 + hh, tc_], cvM[:, hh, tc_], True, True,
                           [t_cvM], [TB[b]])
                    TT(dve, Mt[:].rearrange("p h l -> p (h l)"), Lt[:].rearrange("p h l -> p (h l)"), PS[:, 6 * 512:8 * 512],
                       ALU.mult, [t_Lt, TB[6], TB[7]], [t_Mt])
                    for hh in range(8):
                        b = 1 + hh // 4
                        MM(bank(b)[:, (hh % 4) * 128:(hh % 4 + 1) * 128], Mt[:, hh, :], vtm[:, t, hh * 128:(hh + 1) * 128],
                           True, True, [t_Mt, t_v], [TB[b]])
                    for hh in range(8):
                        b = 6 + hh // 4
                        MM(bank(b)[:, (hh % 4) * 128:(hh % 4 + 1) * 128], cvM[:, hh, tc_], Cmb[:, hh * 128:(hh + 1) * 128],
                           True, True, [t_cvM, t_Cmb], [TB[b]])
                    for hh in range(8):
                        MM(smp[:, 300 + hh:301 + hh], Mt[:, hh, :], onesb[:, 0:1], True, True, [t_Mt, t_cb], [TB[3]])
                    for hh in range(8):
                        MM(smp[:, 308 + hh:309 + hh], cvM[:, hh, tc_], nmb[:, hh:hh + 1], True, True, [t_cvM, t_nmb], [TB[3]])
                    TT(dve, sm[:, 120:128], smp[:, 308:316], sm[:, 96:104], ALU.mult, [TB[3], t_sm], [t_sm])
                    TT(dve, sm[:, 120:128], sm[:, 120:128], smp[:, 300:308], ALU.add, [TB[3], t_sm], [t_sm])
                    TT(dve, fb[:].rearrange("p (h c) -> p h c", h=8), PS[:, 6 * 512:8 * 512].rearrange("p (h c) -> p h c", h=8),
                       bc(sm[:, 96:104].unsqueeze(2), [128, 8, 128]), ALU.mult, [TB[6], TB[7], t_sm], [t_fb])
                    TT(dve, fb[:], PS[:, 1 * 512:3 * 512], fb[:], ALU.add, [TB[1], TB[2], t_fb], [t_fb])
                    RED(sm[:, 128:136], fb[:].rearrange("p (h c) -> p h c", h=8), ALU.add, [t_fb], [t_sm])
                    ACT(fa[:], fb[:], AF.Square, [t_fb], [t_fa])
                    RED(sm[:, 136:144], fa[:].rearrange("p (h c) -> p h c", h=8), ALU.add, [t_fa], [t_sm])
                    TS(dve, sm[:, 128:136], sm[:, 128:136], 1.0 / 128, ALU.mult, [t_sm], [t_sm])
                    TT(dve, sm[:, 144:152], sm[:, 128:136], sm[:, 128:136], ALU.mult, [t_sm], [t_sm])
                    STT(sm[:, 136:144], sm[:, 136:144], 1.0 / 128, sm[:, 144:152], ALU.mult, ALU.subtract, [t_sm], [t_sm])
                    TT(dve, sm[:, 144:152], sm[:, 120:128], sm[:, 120:128], ALU.mult, [t_sm], [t_sm])
                    TS(dve, sm[:, 144:152], sm[:, 144:152], 1.0, ALU.max, [t_sm], [t_sm])
                    STT(sm[:, 136:144], sm[:, 144:152], EPS, sm[:, 136:144], ALU.mult, ALU.add, [t_sm], [t_sm])
                    ACT(sm[:, 136:144], sm[:, 136:144], AF.Ln, [t_sm], [t_sm])
                    ACT(sm[:, 136:144], sm[:, 136:144], AF.Exp, [t_sm], [t_sm], scale=-0.5)
                    TT(dve, fb[:].rearrange("p (h c) -> p h c", h=8), fb[:].rearrange("p (h c) -> p h c", h=8),
                       bc(sm[:, 128:136].unsqueeze(2), [128, 8, 128]), ALU.subtract, [t_fb, t_sm], [t_fb])
                    TT(dve, fb[:].rearrange("p (h c) -> p h c", h=8), fb[:].rearrange("p (h c) -> p h c", h=8),
                       bc(sm[:, 136:144].unsqueeze(2), [128, 8, 128]), ALU.mult, [t_fb, t_sm], [t_fb])
                    TT(pool, fa[:], so[:, t, :], mnw[:], ALU.mult, [t_so, t_nw], [t_fa])
                    TT(dve, ynb[:], fb[:], fa[:], ALU.mult, [t_fb, t_fa], [t_ynb])
                    for cc in range(8):
                        TR(bankbf(0)[:, cc * 128:(cc + 1) * 128], ynb[:, cc * 128:(cc + 1) * 128], identb[:], [t_ynb, t_cb], [TB[0]])
                    CP(act, mixT[:, 8:16, :].rearrange("p a b -> p (a b)"), bankbf(0), [TB[0]], [t_mixT])
                    TT(dve, kk[:].rearrange("p (h c) -> p h c", h=8), k_tm[:].rearrange("p (h c) -> p h c", h=8),
                       bc(sm[:, 104:112].unsqueeze(2), [128, 8, 128]), ALU.mult, [t_ktm, t_sm], [t_kk])
                    for hh in range(8):
                        b = 6 + hh // 4
                        MM(bank(b)[:, (hh % 4) * 128:(hh % 4 + 1) * 128], kk[:, hh * 128:(hh + 1) * 128],
                           vtm[:, t, hh * 128:(hh + 1) * 128], True, True, [t_kk, t_v], [TB[b]])
                    for hh in range(8):
                        MM(smp[:, 320 + hh:321 + hh], kk[:, hh * 128:(hh + 1) * 128], onesb[:, 0:1], True, True,
                           [t_kk, t_cb], [TB[3]])
                    TT(dve, Cm[:].rearrange("p (h c) -> p h c", h=8), Cm[:].rearrange("p (h c) -> p h c", h=8),
                       bc(sm[:, 112:120].unsqueeze(2), [128, 8, 128]), ALU.mult, [t_Cm, t_sm], [t_Cm])
                    TT(dve, Cm[:], Cm[:], PS[:, 6 * 512:8 * 512], ALU.add, [t_Cm, TB[6], TB[7]], [t_Cm])
                    CP(act, Cmb[:], Cm[:], [t_Cm], [t_Cmb])
                    TT(dve, nm[:], nm[:], sm[:, 112:120], ALU.mult, [t_nm, t_sm], [t_nm])
                    TT(dve, nm[:], nm[:], smp[:, 320:328], ALU.add, [t_nm, TB[3]], [t_nm])
                    CP(dve, nmb[:], nm[:], [t_nm], [t_nmb])

                CUT(5)
                CUT(100 + 10 * t + 5)
                if 'out' not in _os.environ.get('KSKIP', ''):
                    tk = tok0 + t * 128
                    for nh in range(2):
                        for e in range(16):
                            MM(bank(4 + nh), mixT[:, e, :], wout[:, e, nh * 512:(nh + 1) * 512], e == 0, e == 15,
                               [t_mixT, t_wout], [TB[4 + nh]])
                    sp.dma(xt[:], x_d[tk:tk + 128, :], W=[t_xt])
                    TT(dve, fa[:], PS[:, 4 * 512:6 * 512], xt[:], ALU.add, [TB[4], TB[5], t_xt], [t_fa])
                    sp.dma(x1_d[tk:tk + 128, :], fa[:], R=[t_fa])
                    ACT(junk[:], fa[:], AF.Square, [t_fa], [t_junk, t_st4], accum_out=st4[:, 10:11])
                    ACT(st4[:, 11:12], st4[:, 10:11], AF.Ln, [t_st4], [t_st4], scale=1.0 / D, bias=EPS)
                    ACT(st4[:, 12:13], st4[:, 11:12], AF.Exp, [t_st4], [t_st4], scale=-0.5)
                    TS(dve, fb[:], fa[:], st4[:, 12:13], ALU.mult, [t_fa, t_st4], [t_fb])
                    CP(pool, xhb[:], fb[:], [t_fb], [t_xhb])
                    sp.dma(h2_d[tk:tk + 128, :], xhb[:], R=[t_xhb])
                    for kc in range(8):
                        b = 1 + kc // 4
                        TR(bank(b)[:, (kc % 4) * 128:(kc % 4 + 1) * 128], fb[:, kc * 128:(kc + 1) * 128], identf, [t_fb, t_c], [TB[b]])
                    TT(dve, h2T[:], PS[:, 1 * 512:3 * 512].rearrange("p (k c) -> p k c", k=8),
                       bc(n2w[:].unsqueeze(2), [128, 8, 128]), ALU.mult, [TB[1], TB[2], t_par], [t_h2T])
                    for kc in range(8):
                        MM(bank(3, 36), h2T[:, kc, :], wr[:, kc, :], kc == 0, kc == 7, [t_h2T, t_par], [TB[3]])
                    TT(dve, lgt[:], bank(3, 36), rb_b, ALU.add, [TB[3], t_rep], [t_lgt])
                    sp.dma(lg_d[tk:tk + 128, :], lgt[:], R=[t_lgt])
                CUT(6)
                CUT(100 + 10 * t + 6)
                if _os.environ.get("KBAR", "0") == "1":
                    K.barrier()

    K.barrier()
    if stop_after == "p1":
        return K
    K.release(ph1)

    IOA = bass.IndirectOffsetOnAxis
    NTL = NTILE
    Wd = NTL * 32
    lg = K.sb([128, NTL, 36], F32, "lg")
    t_lg = Tok()
    sp.dma(lg[:], lg_d.rearrange("(j p) c -> p j c", p=128), W=[t_lg])
    nfw = K.sb([128, D], F32, "nfw")
    t_nfw = Tok()
    sp.dma(nfw[:], nfw_d, W=[t_nfw])

    def rt(shape, dt=F32, name=None):
        return K.sb(shape, dt, name)
    t_r = Tok()
    gts = rt([128, 2, NTL]); desti = rt([128, 2, NTL], I32); idxG = rt([128, NSLOT, 8], I32); idxD = rt([128, NSLOT, 4], I32)
    hj = [K.sb([128, D], BF16, f"hj{i}") for i in range(2)]
    rmark = K.mark()
    gmx = rt([128, NTL]); ohg = rt([128, NTL, 4]); eg = rt([128, NTL, 4]); sg = rt([128, NTL]); pg = rt([128, NTL])
    pen = rt([128, NTL, 4]); me = rt([128, NTL, 32]); me2 = rt([128, NTL, 32]); oh1 = rt([128, NTL, 32]); oh2 = rt([128, NTL, 32])
    v1 = rt([128, NTL]); v2 = rt([128, NTL]); tmpw = rt([128, NTL, 32])
    OHb = rt([128, Wd], BF16); PCs = rt([128, NTL, 32]); Ts = rt([128, NTL, 32]); Rr = rt([128, NTL, 32])
    cnt = rt([128, 32]); cmp_ = rt([128, 32, 64]); nsl = rt([128, 32]); bsl = rt([128, 32]); incl = rt([128, 32]); base = rt([128, 32])
    destf = rt([128, 2, NTL]); cmp2 = rt([128, NSLOT, 32]); eslot = rt([128, NSLOT])
    idxf = rt([128, NSLOT, 8])
    gl = lg[:, :, 0:4]
    el = lg[:, :, 4:36]
    RED(gmx[:], gl, ALU.max, [t_lg], [t_r])
    TT(dve, ohg[:], gl, bc(gmx[:].unsqueeze(2), [128, NTL, 4]), ALU.is_equal, [t_lg, t_r], [t_r])
    TT(dve, eg[:], gl, bc(gmx[:].unsqueeze(2), [128, NTL, 4]), ALU.subtract, [t_lg, t_r], [t_r])
    ACT(eg[:], eg[:], AF.Exp, [t_r], [t_r])
    RED(sg[:], eg[:], ALU.add, [t_r], [t_r])
    dve.do(lambda: nc.vector.reciprocal(out=pg[:], in_=sg[:]), [t_r], [t_r])
    TS(dve, pen[:], ohg[:], -1.0, ALU.add, [t_r], [t_r], s2=1e30, op1=ALU.mult)
    TT(dve, me[:].rearrange("p j (g e) -> p j g e", g=4), el.rearrange("p j (g e) -> p j g e", g=4),
       bc(pen[:].unsqueeze(3), [128, NTL, 4, 8]), ALU.add, [t_lg, t_r], [t_r])
    RED(v1[:], me[:], ALU.max, [t_r], [t_r])
    TT(dve, oh1[:], me[:], bc(v1[:].unsqueeze(2), [128, NTL, 32]), ALU.is_equal, [t_r], [t_r])
    STT(me2[:].rearrange("p j e -> p (j e)"), oh1[:].rearrange("p j e -> p (j e)"), -1e30,
        me[:].rearrange("p j e -> p (j e)"), ALU.mult, ALU.add, [t_r], [t_r])
    RED(v2[:], me2[:], ALU.max, [t_r], [t_r])
    TT(dve, oh2[:], me2[:], bc(v2[:].unsqueeze(2), [128, NTL, 32]), ALU.is_equal, [t_r], [t_r])
    TT(dve, v2[:], v2[:], v1[:], ALU.subtract, [t_r], [t_r])
    ACT(v2[:], v2[:], AF.Exp, [t_r], [t_r])
    TS(dve, v2[:], v2[:], 1.0, ALU.add, [t_r], [t_r])
    dve.do(lambda: nc.vector.reciprocal(out=v2[:], in_=v2[:]), [t_r], [t_r])
    TT(dve, gts[:, 0, :], pg[:], v2[:], ALU.mult, [t_r], [t_r])
    TT(dve, gts[:, 1, :], pg[:], gts[:, 0, :], ALU.subtract, [t_r], [t_r])
    TT(dve, OHb[:], oh1[:].rearrange("p j e -> p (j e)"), oh2[:].rearrange("p j e -> p (j e)"), ALU.add, [t_r], [t_r])
    nchk = (Wd + 511) // 512
    for c in range(nchk):
        w_ = min(512, Wd - c * 512)
        MM(bank(c, w_), ustrb[:], OHb[:, c * 512:c * 512 + w_], True, True, [t_r, t_cb], [TB[c]])
        MM(bank(4 + c, w_), onesb[:], OHb[:, c * 512:c * 512 + w_], True, True, [t_r, t_cb], [TB[4 + c]])
    CP(dve, PCs[:].rearrange("p j e -> p (j e)"), PS[:, 0:Wd], [TB[c_] for c_ in range(nchk)], [t_r])
    CP(dve, Ts[:].rearrange("p j e -> p (j e)"), PS[:, 2048:2048 + Wd], [TB[4 + c_] for c_ in range(nchk)], [t_r])
    dve.do(lambda: nc.vector.memset(Rr[:, 0, :], 0.0), W=[t_r])
    for j in range(1, NTL):
        TT(dve, Rr[:, j, :], Rr[:, j - 1, :], Ts[:, j - 1, :], ALU.add, [t_r], [t_r])
    TT(dve, cnt[:], Rr[:, NTL - 1, :], Ts[:, NTL - 1, :], ALU.add, [t_r], [t_r])
    TT(dve, PCs[:], PCs[:], Rr[:], ALU.add, [t_r], [t_r])
    TT(dve, tmpw[:], oh1[:], PCs[:], ALU.mult, [t_r], [t_r])
    RED(destf[:, 0, :], tmpw[:], ALU.add, [t_r], [t_r])
    TT(dve, tmpw[:], oh2[:], PCs[:], ALU.mult, [t_r], [t_r])
    RED(destf[:, 1, :], tmpw[:], ALU.add, [t_r], [t_r])
    TT(dve, cmp_[:], bc(thr.unsqueeze(1), [128, 32, 64]), bc(cnt[:].unsqueeze(2), [128, 32, 64]), ALU.is_lt, [t_r, t_c], [t_r])
    RED(nsl[:], cmp_[:], ALU.add, [t_r], [t_r])
    dve.do(lambda: nc.vector.memset(bsl[:, 0:1], 0.0), W=[t_r])
    for e in range(1, 32):
        TT(dve, bsl[:, e:e + 1], bsl[:, e - 1:e], nsl[:, e - 1:e], ALU.add, [t_r], [t_r])
    TT(dve, incl[:], bsl[:], nsl[:], ALU.add, [t_r], [t_r])
    TS(dve, base[:], bsl[:], float(SL), ALU.mult, [t_r], [t_r])
    for k, ohk in ((0, oh1), (1, oh2)):
        TT(dve, tmpw[:], ohk[:], bc(base[:].unsqueeze(1), [128, NTL, 32]), ALU.mult, [t_r], [t_r])
        RED(v1[:], tmpw[:], ALU.add, [t_r], [t_r])
        TT(dve, destf[:, k, :], destf[:, k, :], v1[:], ALU.add, [t_r], [t_r])
    CP(dve, desti[:], destf[:], [t_r], [t_r])
    TT(dve, cmp2[:], bc(incl[:].unsqueeze(1), [128, NSLOT, 32]), bc(slotid[:, 0:NSLOT].unsqueeze(2), [128, NSLOT, 32]),
       ALU.is_le, [t_r, t_c], [t_r])
    RED(eslot[:], cmp2[:], ALU.add, [t_r], [t_r])
    TS(dve, eslot[:], eslot[:], 31.0, ALU.min, [t_r], [t_r])
    TS(dve, eslot[:], eslot[:], 128.0, ALU.mult, [t_r], [t_r], s2=rowoff[:, 0:1], op1=ALU.add)
    CP(dve, idxG[:, :, 0], eslot[:], [t_r], [t_r])
    K.barrier()
    K.release(rmark)

    t_scat = []
    t_hj = [Tok() for _ in range(2)]
    for j in range(NTL):
        sp.dma(hj[j % 2][:], h2_d[j * 128:(j + 1) * 128, :], W=[t_hj[j % 2]])
        for k in range(2):
            ts_ = Tok()
            pool.dma(h2s_d[:, :], hj[j % 2][:], R=[t_r, t_hj[j % 2]] + t_zero, W=[ts_],
                     indirect=dict(out_offset=IOA(ap=desti[:, k, j:j + 1], axis=0), in_offset=None))
            t_scat.append(ts_)

    wgb = [K.sb([128, 8, 512], BF16, f"wgb{i}") for i in range(2)]
    wub = [K.sb([128, 8, 512], BF16, f"wub{i}") for i in range(2)]
    wdb = [K.sb([128, 4, D], BF16, f"wdb{i}") for i in range(2)]
    t_w = [Tok() for _ in range(2)]
    hs = K.sb([128, 4, D], BF16, "hs")
    t_hs = Tok()
    hT = K.sb([128, 8, 512], BF16, "hT")
    t_hT = Tok()
    sgb = [K.sb([128, 512], BF16, f"sgb{i}") for i in range(2)]
    t_sgb = [Tok() for _ in range(2)]
    aT = K.sb([128, 4, 512], BF16, "aT")
    t_aT = [Tok() for _ in range(4)]
    yst = [K.sb([128, D], F32, f"yst{i}") for i in range(2)]
    t_yst = [Tok() for _ in range(2)]
    t_ys = []

    def load_w(s):
        q = s % 2
        pool.dma(wgb[q][:].rearrange("p k f -> p (k f)"), wg_d, R=[t_r], W=[t_w[q]],
                 indirect=dict(out_offset=None, in_offset=IOA(ap=idxG[:, s, 0:1], axis=0)))
        pool.dma(wub[q][:].rearrange("p k f -> p (k f)"), wu_d, R=[t_r], W=[t_w[q]],
                 indirect=dict(out_offset=None, in_offset=IOA(ap=idxG[:, s, 0:1], axis=0)))
        pool.dma(wdb[q][:].rearrange("p k f -> p (k f)"), wd_d, R=[t_r], W=[t_w[q]],
                 indirect=dict(out_offset=None, in_offset=IOA(ap=idxG[:, s, 0:1], axis=0)))

    load_w(0)
    yq = 0
    for s in range(NSLOT):
        q = s % 2
        if s + 1 < NSLOT:
            load_w(s + 1)
        sp.dma(hs[:], h2s_d[s * SL:(s + 1) * SL, :].rearrange("(r p) d -> p r d", p=128), R=t_scat, W=[t_hs])
        for r in range(4):
            b0 = 0 if r % 2 == 0 else 7
            for kc in range(8):
                TR(bankbf(b0)[:, kc * 128:(kc + 1) * 128], hs[:, r, kc * 128:(kc + 1) * 128], identb[:], [t_hs, t_cb], [TB[b0]])
            TT(dve, hT[:, :, r * 128:(r + 1) * 128], bankbf(b0).rearrange("p (k c) -> p k c", k=8),
               bc(n2w[:].unsqueeze(2), [128, 8, 128]), ALU.mult, [TB[b0], t_par], [t_hT])
        for fc in range(4):
            bg = [1, 3][fc % 2]
            bu = [2, 6][fc % 2]
            for kc in range(8):
                MM(bank(bg), wgb[q][:, kc, fc * 128:(fc + 1) * 128], hT[:, kc, :], kc == 0, kc == 7, [t_w[q], t_hT], [TB[bg]])
            for kc in range(8):
                MM(bank(bu), wub[q][:, kc, fc * 128:(fc + 1) * 128], hT[:, kc, :], kc == 0, kc == 7, [t_w[q], t_hT], [TB[bu]])
            ACT(sgb[fc % 2][:], bank(bg), AF.Silu, [TB[bg]], [t_sgb[fc % 2]])
            TT(dve, aT[:, fc, :], sgb[fc % 2][:], bank(bu), ALU.mult, [t_sgb[fc % 2], TB[bu]], [t_aT[fc]])
        for r in range(4):
            db = 4 if r % 2 == 0 else 6
            for nh in range(2):
                for fc in range(4):
                    MM(bank(db + nh), aT[:, fc, r * 128:(r + 1) * 128], wdb[q][:, fc, nh * 512:(nh + 1) * 512], fc == 0, fc == 3,
                       [t_aT[fc], t_w[q]], [TB[db + nh]])
            CP(act, yst[yq][:], PS[:, db * 512:(db + 2) * 512], [TB[db], TB[db + 1]], [t_yst[yq]])
            ty = Tok()
            sp.dma(ys_d[s * SL + r * 128: s * SL + (r + 1) * 128, :], yst[yq][:], R=[t_yst[yq]], W=[ty])
            t_ys.append(ty)
            yq ^= 1

    ya = K.sb([128, D], F32, "ya")
    yb = K.sb([128, D], F32, "yb")
    x1t = K.sb([128, D], F32, "x1t")
    acc = K.sb([128, D], F32, "acc")
    ot = K.sb([128, D], F32, "ot")
    jk = K.sb([128, D], BF16, "jk")
    s5 = K.sb([128, 4], F32, "s5")
    t_ya, t_yb, t_x1t, t_acc, t_ot, t_jk, t_s5 = (Tok() for _ in range(7))
    t_out = []
    for j in range(NTL):
        pool.dma(ya[:], ys_d, R=[t_r] + t_ys, W=[t_ya],
                 indirect=dict(out_offset=None, in_offset=IOA(ap=desti[:, 0, j:j + 1], axis=0)))
        pool.dma(yb[:], ys_d, R=[t_r] + t_ys, W=[t_yb],
                 indirect=dict(out_offset=None, in_offset=IOA(ap=desti[:, 1, j:j + 1], axis=0)))
        sp.dma(x1t[:], x1_d[j * 128:(j + 1) * 128, :], W=[t_x1t])
        STT(acc[:], ya[:], gts[:, 0, j:j + 1], x1t[:], ALU.mult, ALU.add, [t_ya, t_x1t, t_r], [t_acc])
        STT(acc[:], yb[:], gts[:, 1, j:j + 1], acc[:], ALU.mult, ALU.add, [t_yb, t_acc, t_r], [t_acc])
        ACT(jk[:], acc[:], AF.Square, [t_acc], [t_jk, t_s5], accum_out=s5[:, 0:1])
        ACT(s5[:, 1:2], s5[:, 0:1], AF.Ln, [t_s5], [t_s5], scale=1.0 / D, bias=EPS)
        ACT(s5[:, 2:3], s5[:, 1:2], AF.Exp, [t_s5], [t_s5], scale=-0.5)
        STT(ot[:], acc[:], s5[:, 2:3], nfw[:], ALU.mult, ALU.mult, [t_acc, t_s5, t_nfw], [t_ot])
        to = Tok()
        sp.dma(out_d[j * 128:(j + 1) * 128, :], ot[:], R=[t_ot], W=[to])
        t_out.append(to)
    K.barrier()
    return K


def _shared_inputs(inp):
    f = lambda a: np.ascontiguousarray(np.asarray(a, dtype=np.float32))
    w_in = f(inp["w_in"])[0]
    order = np.concatenate([np.arange(1024, 2560), np.arange(0, 1024), np.arange(2560, 2576),
                            np.arange(6672, 6688), np.arange(2576, 4624), np.arange(4624, 5648),
                            np.arange(5648, 6672)])
    w_in_p = w_in[:, order].reshape(8, 128, NCOLS).transpose(1, 0, 2)
    w_out = f(inp["w_out"])[0].reshape(16, 128, D).transpose(1, 0, 2)
    cwf = np.concatenate([f(inp["ssd_conv_w"])[0], f(inp["ml_conv_w"])[0]], axis=1)
    cw = cwf.T.reshape(28, 128, 4).transpose(1, 0, 2)
    cbf = np.concatenate([f(inp["ssd_conv_b"])[0], f(inp["ml_conv_b"])[0]])
    cb = cbf.reshape(28, 128).T
    n1w = f(inp["norm1_w"])[0].reshape(8, 128).T
    n2w = f(inp["norm2_w"])[0].reshape(8, 128).T
    repv = np.concatenate([f(inp["ssd_dt_bias"])[0], f(inp["ssd_a_log"])[0], f(inp["ssd_d"])[0],
                           f(inp["ml_i_bias"])[0], f(inp["ml_f_bias"])[0],
                           f(inp["router_g_b"])[0], f(inp["router_e_b"])[0]])
    rep = np.tile(repv[None, :], (128, 1))
    snw = np.tile(f(inp["ssd_norm_w"])[0][None, :], (128, 1))
    mnw = np.tile(f(inp["ml_norm_w"])[0][None, :], (128, 1))
    nfw = np.tile(f(inp["norm_f_w"])[None, :], (128, 1))
    wr = np.concatenate([f(inp["router_g_w"])[0], f(inp["router_e_w"])[0]], axis=1).reshape(8, 128, 36).transpose(1, 0, 2)
    s = np.arange(128)
    ident = np.eye(128, dtype=np.float32)
    tri = (s[:, None] <= s[None, :]).astype(np.float32)
    ones = np.ones((128, 128), np.float32)
    ustr = (s[:, None] < s[None, :]).astype(np.float32)
    sel = np.zeros((128, 8, 128), np.float32)
    for h in range(8):
        sel[h, h, :] = 1.0
    thr = np.tile((np.arange(64, dtype=np.float32) * SL)[None, :], (128, 1))
    slotid = np.tile(np.arange(128, dtype=np.float32)[None, :], (128, 1))
    rowoff = (np.arange(24, dtype=np.float32)[None, :] * 128 + s[:, None]).astype(np.float32)
    cst = np.concatenate([ident, tri, ones, ustr, sel.reshape(128, 1024), thr, slotid, rowoff], axis=1)
    d = dict(w_in=w_in_p, w_out=w_out, cw=cw, cb=cb, n1w=n1w, n2w=n2w, rep=rep, snw=snw, mnw=mnw, nfw=nfw,
             wr=wr, cst=cst,
             wg=f(inp["exp_w_gate"])[0].reshape(NE, 8, 128, 512).transpose(0, 2, 1, 3).reshape(NE * 128, 8 * 512),
             wu=f(inp["exp_w_up"])[0].reshape(NE, 8, 128, 512).transpose(0, 2, 1, 3).reshape(NE * 128, 8 * 512),
             wd=f(inp["exp_w_down"])[0].reshape(NE, 4, 128, D).transpose(0, 2, 1, 3).reshape(NE * 128, 4 * D))
    return {k: np.ascontiguousarray(v, dtype=np.float32) for k, v in d.items()}


def kernel(**inputs):
    x = np.asarray(inputs["x"], dtype=np.float32)
    B, L, _ = x.shape
    ncores = 8
    nseq = B // ncores
    shared = _shared_inputs(inputs)
    K = build(nseq, L)
    in_maps = []
    for c in range(ncores):
        m = dict(shared)
        m["x"] = np.ascontiguousarray(x[c * nseq:(c + 1) * nseq].reshape(nseq * L, D))
        in_maps.append(m)
    res = run_bass_kernel_spmd(K.nc, in_maps, core_ids=list(range(ncores)))
    outs = [np.asarray(r["out"], dtype=np.float32).reshape(nseq, L, D) for r in res.results]
    return np.concatenate(outs, axis=0)
```

```python
import numpy as np
import concourse.bass as bass
import concourse.mybir as mybir
from concourse.bass_utils import run_bass_kernel_spmd

F32 = mybir.dt.float32
BF16 = mybir.dt.bfloat16
I32 = mybir.dt.int32
AF = mybir.ActivationFunctionType
ALU = mybir.AluOpType
AX = mybir.AxisListType

D = 1024
NCOLS = 6688
EPS = 1e-6
NEG = -30000.0
SL = 512
NE = 32


LOG = []


class Tok:
    __slots__ = ("w", "r", "excl")

    def __init__(self, excl=False):
        self.w = None
        self.r = []
        self.excl = excl


class Chan:
    def __init__(self, nc, name):
        self.sem = nc.alloc_semaphore(name=name)
        self.cnt = 0

    def value_for(self, seq):
        return self.sem, seq


class Eng:
    def __init__(self, nc, name, h, raw_safe=False):
        self.nc = nc
        self.name = name
        self.h = h
        self.sem = nc.alloc_semaphore(name="se_" + name)
        self.cnt = 0
        self.seq = 0
        self.last = None
        self.incs = []
        self.seen = {}
        self.raw_safe = raw_safe
        self.chans = []
        self.chan_i = 0
        self.nwaits = 0
        self.ninst = 0

    def value_for(self, seq):
        best = None
        for s, v in reversed(self.incs):
            if s >= seq:
                best = v
            else:
                break
        if best is not None:
            return self.sem, best
        assert self.last is not None and self.seq >= seq
        self.last.then_inc(self.sem, 1)
        self.cnt += 1
        LOG.append((self.name, "inc", self.seq, self.cnt))
        self.incs.append((self.seq, self.cnt))
        if len(self.incs) > 256:
            self.incs = self.incs[-128:]
        return self.sem, self.cnt

    def wait_on(self, dep):
        src, seq = dep
        if src is self and self.raw_safe:
            return
        sem, val = src.value_for(seq)
        if self.seen.get(sem, 0) >= val:
            return
        self.h.wait_ge(sem, val)
        LOG.append((self.name, "wait", str(sem), val))
        self.nwaits += 1
        self.seen[sem] = val

    def _deps(self, R, W):
        for t in R:
            if t.w is not None:
                self.wait_on(t.w)
            if t.excl:
                for d in t.r:
                    if d[0] is not self:
                        self.wait_on(d)
        for t in W:
            if t.w is not None and t.w[0] is not self:
                self.wait_on(t.w)
            for d in t.r:
                if d[0] is not self:
                    self.wait_on(d)

    def do(self, fn, R=(), W=()):
        self._deps(R, W)
        inst = fn()
        self.seq += 1
        self.ninst += 1
        self.last = inst
        LOG.append((self.name, "inst", self.seq, type(inst).__name__))
        me = (self, self.seq)
        for t in R:
            t.r.append(me)
        for t in W:
            t.w = me
            t.r = []
        return inst

    def dma(self, out, in_, R=(), W=(), indirect=None):
        ch = self.chans[self.chan_i]
        self.chan_i = (self.chan_i + 1) % len(self.chans)
        if ch.cnt and self.seen.get(ch.sem, 0) < ch.cnt:
            self.h.wait_ge(ch.sem, ch.cnt)
            self.seen[ch.sem] = ch.cnt
        self._deps(R, W)
        if indirect is None:
            inst = self.h.dma_start(out=out, in_=in_)
        else:
            inst = self.h.indirect_dma_start(out=out, in_=in_, **indirect)
        inst.then_inc(ch.sem, 16)
        ch.cnt += 16
        self.ninst += 1
        me = (ch, ch.cnt)
        for t in R:
            t.r.append(me)
        for t in W:
            t.w = me
            t.r = []
        return inst


class KB:
    def __init__(self):
        self.nc = bass.Bass("TRN2", target_bir_lowering=False)
        nc = self.nc
        self.pe = Eng(nc, "pe", nc.tensor, raw_safe=True)
        self.act = Eng(nc, "act", nc.scalar)
        self.dve = Eng(nc, "dve", nc.vector)
        self.pool = Eng(nc, "pool", nc.gpsimd)
        self.sp = Eng(nc, "sp", nc.sync)
        self.sp.chans = [Chan(nc, f"c_sp{i}") for i in range(8)]
        self.pool.chans = [Chan(nc, f"c_pl{i}") for i in range(8)]
        self.engs = [self.pe, self.act, self.dve, self.pool, self.sp]
        self._stack = []
        self._names = 0

    def sb(self, shape, dt, name=None):
        self._names += 1
        g = self.nc.sbuf_tensor(f"s{self._names}_{name or 'sb'}", list(shape), dt)
        t = g.__enter__()
        self._stack.append(g)
        return t

    def ps(self, shape, dt, name=None):
        self._names += 1
        g = self.nc.psum_tensor(f"p{self._names}_{name or 'ps'}", list(shape), dt)
        t = g.__enter__()
        self._stack.append(g)
        return t

    def barrier(self):
        LOG.append(("all", "barrier", 0, 0))
        for e in self.engs:
            for o in self.engs:
                if o is not e and o.seq > 0:
                    e.wait_on((o, o.seq))
            for o in self.engs:
                for ch in o.chans:
                    if ch.cnt:
                        e.wait_on((ch, ch.cnt))

    def mark(self):
        return len(self._stack)

    def release(self, mark):
        while len(self._stack) > mark:
            g = self._stack.pop()
            g.__exit__(None, None, None)


class _Cut(Exception):
    pass


def build(NSEQ, L, stop_after=None, cut=None):
    try:
        return _build(NSEQ, L, stop_after, cut)
    except _Cut as e:
        K = e.args[0]
        K.barrier()
        return K


def _build(NSEQ, L, stop_after=None, cut=None):
    K = KB()

    def CUT(n):
        if cut == n:
            raise _Cut(K)

    nc = K.nc
    pe, act, dve, pool, sp = K.pe, K.act, K.dve, K.pool, K.sp
    NT = NSEQ * L
    NTILE = NT // 128
    NBLK = L // 512
    NSLOT = (2 * NT) // SL + NE
    NROW = NSLOT * SL

    def din(name, shape, dt=F32):
        return nc.dram_tensor(name, list(shape), dt, kind="ExternalInput").ap()

    def dscr(name, shape, dt):
        return nc.dram_tensor(name, list(shape), dt, kind="Internal").ap()

    x_d = din("x", [NT, D])
    win_d = din("w_in", [128, 8, NCOLS])
    wout_d = din("w_out", [128, 16, D])
    cw_d = din("cw", [128, 28, 4])
    cb_d = din("cb", [128, 28])
    n1w_d = din("n1w", [128, 8])
    n2w_d = din("n2w", [128, 8])
    rep_d = din("rep", [128, 16 * 3 + 8 * 2 + 36])
    snw_d = din("snw", [128, D])
    mnw_d = din("mnw", [128, D])
    nfw_d = din("nfw", [128, D])
    wr_d = din("wr", [128, 8, 36])
    cst_d = din("cst", [128, 128 * 4 + 1024 + 64 + 128 + 24])
    wg_d = din("wg", [NE * 128, 8 * 512])
    wu_d = din("wu", [NE * 128, 8 * 512])
    wd_d = din("wd", [NE * 128, 4 * D])
    out_d = nc.dram_tensor("out", [NT, D], F32, kind="ExternalOutput").ap()
    x1_d = nc.dram_tensor("x1s", [NT, D], F32, kind=("ExternalOutput" if stop_after else "Internal")).ap()
    lg_d = nc.dram_tensor("lgs", [NT, 36], F32, kind=("ExternalOutput" if stop_after else "Internal")).ap()
    h2_d = dscr("h2s", [NT, D], BF16)
    h2s_d = dscr("h2sorted", [NROW, D], BF16)
    ys_d = dscr("yss", [NROW, D], F32)

    PS = K.ps([128, 4096], F32, "psall")
    TB = [Tok(excl=True) for _ in range(8)]

    def bank(b, n=512, p=128):
        return PS[0:p, b * 512:b * 512 + n]

    def bankbf(b):
        return PS[:, b * 512:(b + 1) * 512].bitcast(BF16)

    def ACT(out, in_, func, R, W, **kw):
        return act.do(lambda: nc.scalar.activation(out=out, in_=in_, func=func, **kw), R, W)

    def TT(e, out, a, b, op, R, W):
        return e.do(lambda: e.h.tensor_tensor(out=out, in0=a, in1=b, op=op), R, W)

    def TS(e, out, a, s1, op0, R, W, s2=None, op1=None):
        if op1 is None:
            return e.do(lambda: e.h.tensor_scalar(out=out, in0=a, scalar1=s1, scalar2=None, op0=op0), R, W)
        return e.do(lambda: e.h.tensor_scalar(out=out, in0=a, scalar1=s1, scalar2=s2, op0=op0, op1=op1), R, W)

    def STT(out, a, s, b, op0, op1, R, W):
        return dve.do(lambda: nc.vector.scalar_tensor_tensor(out=out, in0=a, scalar=s, in1=b, op0=op0, op1=op1), R, W)

    def CP(e, out, in_, R, W):
        if e is act:
            return act.do(lambda: nc.scalar.copy(out=out, in_=in_), R, W)
        return e.do(lambda: e.h.tensor_copy(out=out, in_=in_), R, W)

    def MM(out, lhsT, rhs, start, stop, R, W):
        return pe.do(lambda: nc.tensor.matmul(out, lhsT, rhs, start=start, stop=stop), R, W)

    def TR(out, in_, ident, R, W):
        return pe.do(lambda: nc.tensor.transpose(out, in_, ident), R, W)

    def RED(out, in_, op, R, W, axis=AX.X):
        return dve.do(lambda: nc.vector.tensor_reduce(out=out, in_=in_, axis=axis, op=op), R, W)

    def bc(ap, shape):
        return ap.broadcast_to(list(shape))

    cst = K.sb([128, 128 * 4 + 1024 + 64 + 128 + 24], F32, "cst")
    t_c = Tok()
    sp.dma(cst[:], cst_d, W=[t_c])
    identf = cst[:, 0:128]
    trif = cst[:, 128:256]
    onesf = cst[:, 256:384]
    ustr = cst[:, 384:512]
    sel8 = cst[0:8, 512:1536]
    thr = cst[:, 1536:1600]
    slotid = cst[:, 1600:1728]
    rowoff = cst[:, 1728:1752]
    identb = K.sb([128, 128], BF16, "identb")
    onesb = K.sb([128, 128], BF16, "onesb")
    ustrb = K.sb([128, 128], BF16, "ustrb")
    maskb = K.sb([128, 8, 128], BF16, "maskb")
    t_cb = Tok()
    CP(dve, identb[:], identf, [t_c], [t_cb])
    CP(dve, onesb[:], onesf, [t_c], [t_cb])
    CP(dve, ustrb[:], ustr, [t_c], [t_cb])
    for h in range(8):
        TS(dve, maskb[:, h, :], trif, -1.0, ALU.add, [t_c], [t_cb], s2=-NEG, op1=ALU.mult)

    rep = K.sb([128, 100], F32, "rep")
    t_rep = Tok()
    sp.dma(rep[:], rep_d, W=[t_rep])
    dtb_b = rep[:, 0:16]
    alog_b = rep[:, 16:32]
    dsk_b = rep[:, 32:48]
    ib_b = rep[:, 48:56]
    fb_b = rep[:, 56:64]
    rb_b = rep[:, 64:100]
    a_b = K.sb([128, 16], F32, "a_b")
    t_ab = Tok()
    ACT(a_b[:], alog_b, AF.Exp, [t_rep], [t_ab])
    TS(dve, a_b[:], a_b[:], -1.0, ALU.mult, [t_ab], [t_ab])
    n1w = K.sb([128, 8], F32, "n1w")
    n2w = K.sb([128, 8], F32, "n2w")
    cw = K.sb([128, 28, 4], F32, "cw")
    cbias = K.sb([128, 28], F32, "cbias")
    t_par = Tok()
    sp.dma(n1w[:], n1w_d, W=[t_par])
    sp.dma(n2w[:], n2w_d, W=[t_par])
    sp.dma(cw[:], cw_d, W=[t_par])
    sp.dma(cbias[:], cb_d, W=[t_par])
    wr = K.sb([128, 8, 36], F32, "wr")
    sp.dma(wr[:], wr_d, W=[t_par])
    wsmf = K.sb([128, 8, 32], F32, "wsmf")
    wsm = K.sb([128, 8, 32], BF16, "wsm")
    t_wsm = Tok()
    sp.dma(wsmf[:], win_d[:, :, 2560:2592], W=[t_wsm])
    CP(dve, wsm[:], wsmf[:], [t_wsm], [t_wsm])

    CUT(0)
    ph1 = K.mark()
    snw = K.sb([128, D], BF16, "snw")
    mnw = K.sb([128, D], BF16, "mnw")
    t_nw = Tok()
    pool.dma(snw[:], snw_d, W=[t_nw])
    pool.dma(mnw[:], mnw_d, W=[t_nw])
    dgp = [K.sb([128, 4, 4, 128], BF16, f"dgp{i}") for i in range(2)]
    t_dgp = [Tok() for _ in range(2)]
    t_dg = Tok()
    dpar = [0]
    dgD = K.sb([128, 16, 128], BF16, "dgD")
    for h in range(16):
        TS(dve, dgD[:, h, :], identf, dsk_b[:, h:h + 1], ALU.mult, [t_c, t_rep], [t_dg])
    wout = K.sb([128, 16, D], BF16, "wout")
    t_wout = Tok()
    for e2 in range(4):
        pool.dma(wout[:, e2 * 4:(e2 + 1) * 4, :], wout_d[:, e2 * 4:(e2 + 1) * 4, :], W=[t_wout])

    NWB = 2
    wbuf = [K.sb([128, 8, 512], BF16, f"wbuf{i}") for i in range(NWB)]
    t_wbuf = [Tok() for _ in range(NWB)]
    xt = K.sb([128, D], F32, "xt")
    t_xt = Tok()
    junk = K.sb([128, D], BF16, "junk")
    t_junk = Tok()
    xn = K.sb([128, D], BF16, "xn")
    t_xn = Tok()
    st4 = K.sb([128, 16], F32, "st4")
    t_st4 = Tok()
    hnT = K.sb([128, 8, 512], BF16, "hnT")
    t_hnT = Tok()
    pb = [K.sb([128, 515], BF16, f"pb{i}") for i in range(2)]
    t_pb = [Tok() for _ in range(2)]
    hal = K.sb([128, 28, 4], BF16, "hal")
    t_hal = [Tok() for _ in range(28)]
    cvS = K.sb([128, 12, 512], BF16, "cvS")
    t_cvS = Tok()
    cvM = K.sb([128, 16, 512], BF16, "cvM")
    t_cvM = Tok()
    zs = K.sb([128, 4, D], BF16, "zs")
    t_zs = Tok()
    vtm = K.sb([128, 4, D], BF16, "vtm")
    t_v = Tok()
    so = K.sb([128, 4, D], BF16, "so")
    t_so = Tok()
    gates = K.sb([128, 4, 32], F32, "gates")
    t_g = Tok()
    gs = K.sb([128, 4, 16 * 2 + 8 * 2], F32, "gs")
    t_gs = Tok()
    sm = K.sb([128, 160], F32, "sm")
    t_sm = Tok()
    csT = K.sb([8, 4, 128], F32, "csT")
    t_csT = Tok()
    dve.do(lambda: nc.vector.memset(csT[:, 3, :], -1.0), W=[t_csT])
    bd = K.sb([8, 8, 128], F32, "bd")
    t_bd = Tok()
    Lt = K.sb([128, 8, 128], BF16, "Lt")
    t_Lt = Tok()
    Mt = K.sb([128, 8, 128], BF16, "Mt")
    t_Mt = Tok()
    cbt = K.sb([128, 128], BF16, "cbt")
    t_cbt = Tok()
    xs_tm = K.sb([128, D], BF16, "xs_tm")
    t_xs = Tok()
    xdt_tm = K.sb([128, D], BF16, "xdt_tm")
    t_xdt = Tok()
    xdec = K.sb([128, D], BF16, "xdec")
    t_xdec = Tok()
    B_tm = K.sb([128, 256], BF16, "B_tm")
    t_Btm = Tok()
    k_tm = K.sb([128, D], BF16, "k_tm")
    t_ktm = Tok()
    kk = K.sb([128, D], BF16, "kk")
    t_kk = Tok()
    fa = K.sb([128, D], F32, "fa")
    t_fa = Tok()
    fb = K.sb([128, D], F32, "fb")
    t_fb = Tok()
    ynb = K.sb([128, D], BF16, "ynb")
    t_ynb = Tok()
    mixT = K.sb([128, 16, 128], BF16, "mixT")
    t_mixT = Tok()
    stS = K.sb([128, D], F32, "stS")
    t_stS = Tok()
    stSb = K.sb([128, D], BF16, "stSb")
    t_stSb = Tok()
    Cm = K.sb([128, D], F32, "Cm")
    t_Cm = Tok()
    Cmb = K.sb([128, D], BF16, "Cmb")
    t_Cmb = Tok()
    nm = K.sb([128, 8], F32, "nm")
    t_nm = Tok()
    nmb = K.sb([128, 8], BF16, "nmb")
    t_nmb = Tok()
    xhb = K.sb([128, D], BF16, "xhb")
    t_xhb = Tok()
    h2T = K.sb([128, 8, 128], F32, "h2T")
    t_h2T = Tok()
    lgt = K.sb([128, 36], F32, "lgt")
    t_lgt = Tok()

    pieces = []
    for i in range(3):
        pieces.append(("feat", i * 512, 512, ("S", i * 4)))
    for i in range(2):
        pieces.append(("tok", 1536 + i * 512, 512, ("z", i)))
    pieces.append(("gate", 2560, 32, ("g", 0)))
    for i in range(4):
        pieces.append(("feat", 2592 + i * 512, 512, ("M", i * 4)))
    for i in range(2):
        pieces.append(("tok", 4640 + i * 512, 512, ("v", i)))
    for i in range(2):
        pieces.append(("tok", 5664 + i * 512, 512, ("o", i)))
    NP = len(pieces)
    wq = []
    piece_ctr = [0]

    def issue_piece_load(gi):
        kind, c0, ncl, arg = pieces[gi % NP]
        slot = gi % NWB
        if kind == "gate":
            return
        pool.dma(wbuf[slot][:, :, 0:ncl], win_d[:, :, c0:c0 + ncl], W=[t_wbuf[slot]])

    total_pieces = NSEQ * NBLK * NP
    nxt_load = [0]

    def ensure_loaded(gi):
        while nxt_load[0] < min(total_pieces, gi + NWB):
            issue_piece_load(nxt_load[0])
            nxt_load[0] += 1

    ppar = [0]
    cpar = [0]
    cvpar = [0]

    for seq in range(NSEQ):
        dve.do(lambda: nc.vector.memset(stS[:], 0.0), W=[t_stS])
        dve.do(lambda: nc.vector.memset(stSb[:], 0.0), W=[t_stSb])
        dve.do(lambda: nc.vector.memset(Cm[:], 0.0), W=[t_Cm])
        dve.do(lambda: nc.vector.memset(Cmb[:], 0.0), W=[t_Cmb])
        dve.do(lambda: nc.vector.memset(nm[:], 0.0), W=[t_nm])
        dve.do(lambda: nc.vector.memset(nmb[:], 0.0), W=[t_nmb])
        for ci in range(28):
            pool.do(lambda ci=ci: nc.gpsimd.memset(hal[:, ci, :], 0.0), W=[t_hal[ci]])
        for blk in range(NBLK):
            tok0 = seq * L + blk * 512
            gbase = (seq * NBLK + blk) * NP
            ensure_loaded(gbase)
            for t in range(4):
                sp.dma(xt[:], x_d[tok0 + t * 128: tok0 + (t + 1) * 128, :], W=[t_xt])
                ACT(junk[:], xt[:], AF.Square, [t_xt], [t_junk, t_st4], accum_out=st4[:, 0:1])
                ACT(st4[:, 1:2], st4[:, 0:1], AF.Ln, [t_st4], [t_st4], scale=1.0 / D, bias=EPS)
                ACT(st4[:, 2:3], st4[:, 1:2], AF.Exp, [t_st4], [t_st4], scale=-0.5)
                TS(dve, xn[:], xt[:], st4[:, 2:3], ALU.mult, [t_xt, t_st4], [t_xn])
                b0 = 0 if t % 2 == 0 else 7
                for kc in range(8):
                    TR(bankbf(b0)[:, kc * 128:(kc + 1) * 128], xn[:, kc * 128:(kc + 1) * 128], identb[:],
                       [t_xn, t_cb], [TB[b0]])
                TT(dve, hnT[:, :, t * 128:(t + 1) * 128],
                   bankbf(b0).rearrange("p (k c) -> p k c", k=8),
                   bc(n1w[:].unsqueeze(2), [128, 8, 128]), ALU.mult, [TB[b0], t_par], [t_hnT])
            CUT(1)
            for pi in range(NP):
                gi = gbase + pi
                ensure_loaded(gi)
                kind, c0, ncl, arg = pieces[pi]
                slot = gi % NWB
                wb = wbuf[slot]
                tw = t_wbuf[slot]
                if kind == "gate":
                    wb = wsm
                    tw = t_wsm
                    kind = "tok"
                if kind == "feat":
                    which, ci0 = arg
                    dq = dpar[0]
                    dpar[0] ^= 1
                    for cc in range(4):
                        gci_ = (ci0 + cc) if which == "S" else 12 + ci0 + cc
                        for k in range(4):
                            TS(dve, dgp[dq][:, cc, k, :], identf, cw[:, gci_, k:k + 1], ALU.mult, [t_c, t_par], [t_dgp[dq]])
                    IPB = [1, 2, 4, 5]
                    CVB = [3, 6]

                    def inproj(cc):
                        ci = ci0 + cc
                        gci = ci if which == "S" else 12 + ci
                        b = IPB[ppar[0] % 4]
                        ppar[0] += 1
                        for kc in range(8):
                            MM(bank(b), wb[:, kc, cc * 128:(cc + 1) * 128], hnT[:, kc, :], kc == 0, kc == 7,
                               [tw, t_hnT], [TB[b]])
                        q = cpar[0] % 2
                        cpar[0] += 1
                        CP(pool, pb[q][:, 0:3], hal[:, gci, 0:3], [t_hal[gci]], [t_pb[q]])
                        CP(act, pb[q][:, 3:515], bank(b), [TB[b]], [t_pb[q]])
                        CP(pool, hal[:, gci, 0:3], pb[q][:, 512:515], [t_pb[q]], [t_hal[gci]])
                        return q

                    def conv(cc, q):
                        ci = ci0 + cc
                        gci = ci if which == "S" else 12 + ci
                        cb_ = CVB[cvpar[0] % 2]
                        cvpar[0] += 1
                        for k in range(4):
                            MM(bank(cb_), dgp[dq][:, cc, k, :], pb[q][:, k:k + 512], k == 0, k == 3,
                               [t_dgp[dq], t_pb[q]], [TB[cb_]])
                        if which == "S":
                            ACT(cvS[:, ci, :], bank(cb_), AF.Silu, [TB[cb_], t_par], [t_cvS], bias=cbias[:, gci:gci + 1])
                        else:
                            ACT(cvM[:, ci, :], bank(cb_), AF.Silu, [TB[cb_], t_par], [t_cvM], bias=cbias[:, gci:gci + 1])

                    qprev = inproj(0)
                    for cc in range(1, 4):
                        qn = inproj(cc)
                        conv(cc - 1, qprev)
                        qprev = qn
                    conv(3, qprev)
                else:
                    which, half = arg
                    for t in range(4):
                        b = [1, 2, 4, 5][ppar[0] % 4]
                        ppar[0] += 1
                        for kc in range(8):
                            MM(bank(b, ncl), hnT[:, kc, t * 128:(t + 1) * 128], wb[:, kc, 0:ncl], kc == 0, kc == 7,
                               [tw, t_hnT], [TB[b]])
                        if which == "z":
                            ACT(zs[:, t, half * 512:(half + 1) * 512], bank(b), AF.Silu, [TB[b]], [t_zs])
                        elif which == "v":
                            CP(act, vtm[:, t, half * 512:(half + 1) * 512], bank(b), [TB[b]], [t_v])
                        elif which == "o":
                            ACT(so[:, t, half * 512:(half + 1) * 512], bank(b), AF.Sigmoid, [TB[b]], [t_so])
                        else:
                            CP(dve, gates[:, t, :], bank(b, 32), [TB[b]], [t_g])
                CUT(10 + pi)
            CUT(2)
            dtv = gs[:, :, 0:16]
            adt = gs[:, :, 16:32]
            spv = gs[:, :, 32:40]
            ipv = gs[:, :, 40:48]
            TT(dve, dtv, gates[:, :, 0:16], bc(dtb_b.unsqueeze(1), [128, 4, 16]), ALU.add, [t_g, t_rep], [t_gs])
            ACT(dtv, dtv, AF.Exp, [t_gs], [t_gs])
            ACT(dtv, dtv, AF.Ln, [t_gs], [t_gs], bias=1.0)
            TT(dve, adt, dtv, bc(a_b[:].unsqueeze(1), [128, 4, 16]), ALU.mult, [t_gs, t_ab], [t_gs])
            TT(dve, spv, gates[:, :, 24:32], bc(fb_b.unsqueeze(1), [128, 4, 8]), ALU.add, [t_g, t_rep], [t_gs])
            ACT(spv, spv, AF.Exp, [t_gs], [t_gs], scale=-1.0)
            ACT(spv, spv, AF.Ln, [t_gs], [t_gs], bias=1.0)
            TT(dve, ipv, gates[:, :, 16:24], bc(ib_b.unsqueeze(1), [128, 4, 8]), ALU.add, [t_g, t_rep], [t_gs])
            TS(dve, ipv, ipv, float(np.log(128.0 ** -0.5)), ALU.add, [t_gs], [t_gs])

            CUT(3)
            import os as _os
            for t in range(int(_os.environ.get("KT0", "0")), 4):
                tc_ = slice(t * 128, (t + 1) * 128)
                for cc in range(8):
                    TR(bankbf(0)[:, cc * 128:(cc + 1) * 128], cvS[:, cc, tc_], identb[:], [t_cvS, t_cb], [TB[0]])
                CUT(200 + 10 * t + 0)
                CP(act, xs_tm[:], bankbf(0), [TB[0]], [t_xs])
                CUT(200 + 10 * t + 7)
                TT(dve, xdt_tm[:].rearrange("p (h c) -> p h c", h=16), xs_tm[:].rearrange("p (h c) -> p h c", h=16),
                   bc(gs[:, t, 0:16].unsqueeze(2), [128, 16, 64]), ALU.mult, [t_xs, t_gs], [t_xdt])
                for g in range(2):
                    TR(bankbf(0)[:, g * 128:(g + 1) * 128], cvS[:, 8 + g, tc_], identb[:], [t_cvS, t_cb], [TB[0]])
                CP(act, B_tm[:], bankbf(0)[:, 0:256], [TB[0]], [t_Btm])
                CUT(200 + 10 * t + 1)
                smp = bank(3)
                MM(smp[:, 0:16], trif, gs[:, t, 16:32], True, True, [t_c, t_gs], [TB[3]])
                MM(smp[:, 16:32], onesf, gs[:, t, 16:32], True, True, [t_c, t_gs], [TB[3]])
                for g in range(2):
                    MM(smp[0:8, 32 + g * 128: 160 + g * 128], gs[:, t, 16 + g * 8:24 + g * 8], trif, True, True,
                       [t_c, t_gs], [TB[3]])
                CP(dve, sm[:, 0:32], smp[:, 0:32], [TB[3]], [t_sm])
                TS(dve, csT[:, 0:2, :].rearrange("p a b -> p (a b)"), smp[0:8, 32:288], -1.0, ALU.mult, [TB[3]], [t_csT])
                ACT(sm[:, 32:48], sm[:, 0:16], AF.Exp, [t_sm], [t_sm])
                TT(dve, sm[:, 48:64], sm[:, 16:32], sm[:, 0:16], ALU.subtract, [t_sm], [t_sm])
                ACT(sm[:, 48:64], sm[:, 48:64], AF.Exp, [t_sm], [t_sm])
                ACT(sm[:, 64:80], sm[:, 16:32], AF.Exp, [t_sm], [t_sm])
                CUT(200 + 10 * t + 2)
                for g in range(2):
                    MM(bank(6 + g), cvS[:, 10 + g, tc_], stSb[:, g * 512:(g + 1) * 512], True, True,
                       [t_cvS, t_stSb], [TB[6 + g]])
                CUT(200 + 10 * t + 3)
                for g in range(2):
                    TT(dve, bd[:], sel8.rearrange("p (h l) -> p h l", h=8),
                       bc(csT[:, g, :].unsqueeze(1), [8, 8, 128]), ALU.mult, [t_c, t_csT], [t_bd])
                    for hb in range(2):
                        eb_ = bank(4 + hb)
                        cs_ = slice(hb * 512, (hb + 1) * 512)
                        MM(eb_, csT[:, g, :], sel8[:, cs_], True, False, [t_csT, t_c], [TB[4 + hb]])
                        MM(eb_, csT[:, 3, :], bd[:].rearrange("p h l -> p (h l)")[:, cs_], False, False,
                           [t_bd, t_csT], [TB[4 + hb]])
                        MM(eb_, identb[:], maskb[:, hb * 4:(hb + 1) * 4, :].rearrange("p h l -> p (h l)"), False, True,
                           [t_cb], [TB[4 + hb]])
                    ACT(Lt[:].rearrange("p h l -> p (h l)"), PS[:, 4 * 512:6 * 512], AF.Exp, [TB[4], TB[5]], [t_Lt])
                    if g == 1:
                        CUT(200 + 10 * t + 4)
                    MM(bank(3, 128), cvS[:, 8 + g, tc_], cvS[:, 10 + g, tc_], True, True, [t_cvS], [TB[3]])
                    CP(act, cbt[:], bank(3, 128), [TB[3]], [t_cbt])
                    TT(dve, Mt[:], Lt[:], bc(cbt[:].unsqueeze(1), [128, 8, 128]), ALU.mult, [t_Lt, t_cbt], [t_Mt])
                    for hh in range(8):
                        h = g * 8 + hh
                        ob = bank(1 + g)[:, hh * 64:(hh + 1) * 64]
                        MM(ob, Mt[:, hh, :], xdt_tm[:, h * 64:(h + 1) * 64], True, False, [t_Mt, t_xdt], [TB[1 + g]])
                        MM(ob, dgD[:, h, :], xs_tm[:, h * 64:(h + 1) * 64], False, True, [t_dg, t_xs], [TB[1 + g]])
                for hh in range(8):
                    TR(bankbf(0)[:, hh * 128:(hh + 1) * 128], cvM[:, 8 + hh, tc_], identb[:], [t_cvM, t_cb], [TB[0]])
                CP(act, k_tm[:], bankbf(0), [TB[0]], [t_ktm])
                smp = bank(3)
                MM(smp[:, 0:8], trif, gs[:, t, 32:40], True, True, [t_c, t_gs], [TB[3]])
                MM(smp[:, 8:16], onesf, gs[:, t, 32:40], True, True, [t_c, t_gs], [TB[3]])
                MM(smp[0:8, 32:160], gs[:, t, 40:48], identf, True, False, [t_c, t_gs], [TB[3]])
                MM(smp[0:8, 32:160], gs[:, t, 32:40], trif, False, True, [t_c, t_gs], [TB[3]])
                MM(smp[0:8, 160:288], gs[:, t, 32:40], trif, True, True, [t_c, t_gs], [TB[3]])
                CP(dve, sm[:, 80:96], smp[:, 0:16], [TB[3]], [t_sm])
                CP(dve, csT[:, 2, :], smp[0:8, 32:160], [TB[3]], [t_csT])
                ACT(sm[:, 96:104], sm[:, 80:88], AF.Exp, [t_sm], [t_sm], scale=-1.0)
                TT(dve, sm[:, 104:112], sm[:, 80:88], sm[:, 88:96], ALU.subtract, [t_sm], [t_sm])
                TT(dve, sm[:, 104:112], sm[:, 104:112], gs[:, t, 40:48], ALU.add, [t_sm, t_gs], [t_sm])
                ACT(sm[:, 104:112], sm[:, 104:112], AF.Exp, [t_sm], [t_sm])
                ACT(sm[:, 112:120], sm[:, 88:96], AF.Exp, [t_sm], [t_sm], scale=-1.0)
                TT(dve, bd[:], sel8.rearrange("p (h l) -> p h l", h=8),
                   bc(smp[0:8, 160:288].unsqueeze(1), [8, 8, 128]), ALU.mult, [t_c, TB[3]], [t_bd])
                for hb in range(2):
                    eb_ = bank(4 + hb)
                    cs_ = slice(hb * 512, (hb + 1) * 512)
                    MM(eb_, csT[:, 2, :], sel8[:, cs_], True, False, [t_csT, t_c], [TB[4 + hb]])
                    MM(eb_, csT[:, 3, :], bd[:].rearrange("p h l -> p (h l)")[:, cs_], False, False,
                       [t_bd, t_csT], [TB[4 + hb]])
                    MM(eb_, identb[:], maskb[:, hb * 4:(hb + 1) * 4, :].rearrange("p h l -> p (h l)"), False, True,
                       [t_cb], [TB[4 + hb]])
                ACT(Lt[:].rearrange("p h l -> p (h l)"), PS[:, 4 * 512:6 * 512], AF.Exp, [TB[4], TB[5]], [t_Lt])
                CUT(200 + 10 * t + 5)
                TT(dve, fa[:].rearrange("p (h c) -> p h c", h=16), PS[:, 6 * 512:8 * 512].rearrange("p (h c) -> p h c", h=16),
                   bc(sm[:, 32:48].unsqueeze(2), [128, 16, 64]), ALU.mult, [TB[6], TB[7], t_sm], [t_fa])
                TT(dve, fa[:], PS[:, 1 * 512:3 * 512], fa[:], ALU.add, [TB[1], TB[2], t_fa], [t_fa])
                TT(dve, fa[:], fa[:], zs[:, t, :], ALU.mult, [t_fa, t_zs], [t_fa])
                for g in range(2):
                    ACT(junk[:, g * 512:(g + 1) * 512], fa[:, g * 512:(g + 1) * 512], AF.Square, [t_fa], [t_junk, t_st4],
                        accum_out=st4[:, 4 + g:5 + g])
                ACT(st4[:, 6:8], st4[:, 4:6], AF.Ln, [t_st4], [t_st4], scale=1.0 / 512, bias=EPS)
                ACT(st4[:, 8:10], st4[:, 6:8], AF.Exp, [t_st4], [t_st4], scale=-0.5)
                for g in range(2):
                    STT(ynb[:, g * 512:(g + 1) * 512], fa[:, g * 512:(g + 1) * 512], st4[:, 8 + g:9 + g],
                        snw[:, g * 512:(g + 1) * 512], ALU.mult, ALU.mult, [t_fa, t_st4, t_nw], [t_ynb])
                for cc in range(8):
                    TR(bankbf(0)[:, cc * 128:(cc + 1) * 128], ynb[:, cc * 128:(cc + 1) * 128], identb[:], [t_ynb, t_cb], [TB[0]])
                CP(act, mixT[:, 0:8, :].rearrange("p a b -> p (a b)"), bankbf(0), [TB[0]], [t_mixT])
                CUT(200 + 10 * t + 6)
                TT(dve, xdec[:].rearrange("p (h c) -> p h c", h=16), xdt_tm[:].rearrange("p (h c) -> p h c", h=16),
                   bc(sm[:, 48:64].unsqueeze(2), [128, 16, 64]), ALU.mult, [t_xdt, t_sm], [t_xdec])
                for g in range(2):
                    MM(bank(6 + g), B_tm[:, g * 128:(g + 1) * 128], xdec[:, g * 512:(g + 1) * 512], True, True,
                       [t_Btm, t_xdec], [TB[6 + g]])
                TT(dve, stS[:].rearrange("p (h c) -> p h c", h=16), stS[:].rearrange("p (h c) -> p h c", h=16),
                   bc(sm[:, 64:80].unsqueeze(2), [128, 16, 64]), ALU.mult, [t_stS, t_sm], [t_stS])
                TT(dve, stS[:], stS[:], PS[:, 6 * 512:8 * 512], ALU.add, [t_stS, TB[6], TB[7]], [t_stS])
                CP(act, stSb[:], stS[:], [t_stS], [t_stSb])

                CUT(4)
                CUT(100 + 10 * t + 4)
                if 'ml' not in _os.environ.get('KSKIP', ''):
                    for hh in range(8):
                        b = 6 + hh // 4
                        MM(bank(b)[:, (hh % 4) * 128:(hh % 4 + 1) * 128], cvM[:, 8 + hh, tc_], cvM[:, hh, tc_], True, True,
                           [t_cvM], [TB[b]])
                    TT(dve, Mt[:].rearrange("p h l -> p (h l)"), Lt[:].rearrange("p h l -> p (h l)"), PS[:, 6 * 512:8 * 512],
                       ALU.mult, [t_Lt, TB[6], TB[7]], [t_Mt])
                    for hh in range(8):
                        b = 1 + hh // 4
                        MM(bank(b)[:, (hh % 4) * 128:(hh % 4 + 1) * 128], Mt[:, hh, :], vtm[:, t, hh * 128:(hh + 1) * 128],
                           True, True, [t_Mt, t_v], [TB[b]])
                    for hh in range(8):
                        b = 6 + hh // 4
                        MM(bank(b)[:, (hh % 4) * 128:(hh % 4 + 1) * 128], cvM[:, hh, tc_], Cmb[:, hh * 128:(hh + 1) * 128],
                           True, True, [t_cvM, t_Cmb], [TB[b]])
                    for hh in range(8):
                        MM(smp[:, 300 + hh:301 + hh], Mt[:, hh, :], onesb[:, 0:1], True, True, [t_Mt, t_cb], [TB[3]])
                    for hh in range(8):
                        MM(smp[:, 308 + hh:309 + hh], cvM[:, hh, tc_], nmb[:, hh:hh + 1], True, True, [t_cvM, t_nmb], [TB[3]])
                    TT(dve, sm[:, 120:128], smp[:, 308:316], sm[:, 96:104], ALU.mult, [TB[3], t_sm], [t_sm])
                    TT(dve, sm[:, 120:128], sm[:, 120:128], smp[:, 300:308], ALU.add, [TB[3], t_sm], [t_sm])
                    TT(dve, fb[:].rearrange("p (h c) -> p h c", h=8), PS[:, 6 * 512:8 * 512].rearrange("p (h c) -> p h c", h=8),
                       bc(sm[:, 96:104].unsqueeze(2), [128, 8, 128]), ALU.mult, [TB[6], TB[7], t_sm], [t_fb])
                    TT(dve, fb[:], PS[:, 1 * 512:3 * 512], fb[:], ALU.add, [TB[1], TB[2], t_fb], [t_fb])
                    RED(sm[:, 128:136], fb[:].rearrange("p (h c) -> p h c", h=8), ALU.add, [t_fb], [t_sm])
                    ACT(fa[:], fb[:], AF.Square, [t_fb], [t_fa])
                    RED(sm[:, 136:144], fa[:].rearrange("p (h c) -> p h c", h=8), ALU.add, [t_fa], [t_sm])
                    TS(dve, sm[:, 128:136], sm[:, 128:136], 1.0 / 128, ALU.mult, [t_sm], [t_sm])
                    TT(dve, sm[:, 144:152], sm[:, 128:136], sm[:, 128:136], ALU.mult, [t_sm], [t_sm])
                    STT(sm[:, 136:144], sm[:, 136:144], 1.0 / 128, sm[:, 144:152], ALU.mult, ALU.subtract, [t_sm], [t_sm])
                    TT(dve, sm[:, 144:152], sm[:, 120:128], sm[:, 120:128], ALU.mult, [t_sm], [t_sm])
                    TS(dve, sm[:, 144:152], sm[:, 144:152], 1.0, ALU.max, [t_sm], [t_sm])
                    STT(sm[:, 136:144], sm[:, 144:152], EPS, sm[:, 136:144], ALU.mult, ALU.add, [t_sm], [t_sm])
                    ACT(sm[:, 136:144], sm[:, 136:144], AF.Ln, [t_sm], [t_sm])
                    ACT(sm[:, 136:144], sm[:, 136:144], AF.Exp, [t_sm], [t_sm], scale=-0.5)
                    TT(dve, fb[:].rearrange("p (h c) -> p h c", h=8), fb[:].rearrange("p (h c) -> p h c", h=8),
                       bc(sm[:, 128:136].unsqueeze(2), [128, 8, 128]), ALU.subtract, [t_fb, t_sm], [t_fb])
                    TT(dve, fb[:].rearrange("p (h c) -> p h c", h=8), fb[:].rearrange("p (h c) -> p h c", h=8),
                       bc(sm[:, 136:144].unsqueeze(2), [128, 8, 128]), ALU.mult, [t_fb, t_sm], [t_fb])
                    TT(pool, fa[:], so[:, t, :], mnw[:], ALU.mult, [t_so, t_nw], [t_fa])
                    TT(dve, ynb[:], fb[:], fa[:], ALU.mult, [t_fb, t_fa], [t_ynb])
                    for cc in range(8):
                        TR(bankbf(0)[:, cc * 128:(cc + 1) * 128], ynb[:, cc * 128:(cc + 1) * 128], identb[:], [t_ynb, t_cb], [TB[0]])
                    CP(act, mixT[:, 8:16, :].rearrange("p a b -> p (a b)"), bankbf(0), [TB[0]], [t_mixT])
                    TT(dve, kk[:].rearrange("p (h c) -> p h c", h=8), k_tm[:].rearrange("p (h c) -> p h c", h=8),
                       bc(sm[:, 104:112].unsqueeze(2), [128, 8, 128]), ALU.mult, [t_ktm, t_sm], [t_kk])
                    for hh in range(8):
                        b = 6 + hh // 4
                        MM(bank(b)[:, (hh % 4) * 128:(hh % 4 + 1) * 128], kk[:, hh * 128:(hh + 1) * 128],
                           vtm[:, t, hh * 128:(hh + 1) * 128], True, True, [t_kk, t_v], [TB[b]])
                    for hh in range(8):
                        MM(smp[:, 320 + hh:321 + hh], kk[:, hh * 128:(hh + 1) * 128], onesb[:, 0:1], True, True,
                           [t_kk, t_cb], [TB[3]])
                    TT(dve, Cm[:].rearrange("p (h c) -> p h c", h=8), Cm[:].rearrange("p (h c) -> p h c", h=8),
                       bc(sm[:, 112:120].unsqueeze(2), [128, 8, 128]), ALU.mult, [t_Cm, t_sm], [t_Cm])
                    TT(dve, Cm[:], Cm[:], PS[:, 6 * 512:8 * 512], ALU.add, [t_Cm, TB[6], TB[7]], [t_Cm])
                    CP(act, Cmb[:], Cm[:], [t_Cm], [t_Cmb])
                    TT(dve, nm[:], nm[:], sm[:, 112:120], ALU.mult, [t_nm, t_sm], [t_nm])
                    TT(dve, nm[:], nm[:], smp[:, 320:328], ALU.add, [t_nm, TB[3]], [t_nm])
                    CP(dve, nmb[:], nm[:], [t_nm], [t_nmb])

                CUT(5)
                CUT(100 + 10 * t + 5)
                if 'out' not in _os.environ.get('KSKIP', ''):
                    tk = tok0 + t * 128
                    for nh in range(2):
                        for e in range(16):
                            MM(bank(4 + nh), mixT[:, e, :], wout[:, e, nh * 512:(nh + 1) * 512], e == 0, e == 15,
                               [t_mixT, t_wout], [TB[4 + nh]])
                    sp.dma(xt[:], x_d[tk:tk + 128, :], W=[t_xt])
                    TT(dve, fa[:], PS[:, 4 * 512:6 * 512], xt[:], ALU.add, [TB[4], TB[5], t_xt], [t_fa])
                    sp.dma(x1_d[tk:tk + 128, :], fa[:], R=[t_fa])
                    ACT(junk[:], fa[:], AF.Square, [t_fa], [t_junk, t_st4], accum_out=st4[:, 10:11])
                    ACT(st4[:, 11:12], st4[:, 10:11], AF.Ln, [t_st4], [t_st4], scale=1.0 / D, bias=EPS)
                    ACT(st4[:, 12:13], st4[:, 11:12], AF.Exp, [t_st4], [t_st4], scale=-0.5)
                    TS(dve, fb[:], fa[:], st4[:, 12:13], ALU.mult, [t_fa, t_st4], [t_fb])
                    CP(pool, xhb[:], fb[:], [t_fb], [t_xhb])
                    sp.dma(h2_d[tk:tk + 128, :], xhb[:], R=[t_xhb])
                    for kc in range(8):
                        b = 1 + kc // 4
                        TR(bank(b)[:, (kc % 4) * 128:(kc % 4 + 1) * 128], fb[:, kc * 128:(kc + 1) * 128], identf, [t_fb, t_c], [TB[b]])
                    TT(dve, h2T[:], PS[:, 1 * 512:3 * 512].rearrange("p (k c) -> p k c", k=8),
                       bc(n2w[:].unsqueeze(2), [128, 8, 128]), ALU.mult, [TB[1], TB[2], t_par], [t_h2T])
                    for kc in range(8):
                        MM(bank(3, 36), h2T[:, kc, :], wr[:, kc, :], kc == 0, kc == 7, [t_h2T, t_par], [TB[3]])
                    TT(dve, lgt[:], bank(3, 36), rb_b, ALU.add, [TB[3], t_rep], [t_lgt])
                    sp.dma(lg_d[tk:tk + 128, :], lgt[:], R=[t_lgt])
                CUT(6)
                CUT(100 + 10 * t + 6)
                if _os.environ.get("KBAR", "0") == "1":
                    K.barrier()

    K.barrier()
    if stop_after == "p1":
        return K
    K.release(ph1)

    IOA = bass.IndirectOffsetOnAxis
    NTL = NTILE
    Wd = NTL * 32
    lg = K.sb([128, NTL, 36], F32, "lg")
    t_lg = Tok()
    sp.dma(lg[:], lg_d.rearrange("(j p) c -> p j c", p=128), W=[t_lg])
    nfw = K.sb([128, D], F32, "nfw")
    t_nfw = Tok()
    sp.dma(nfw[:], nfw_d, W=[t_nfw])
    zt = K.sb([128, 2048], BF16, "zt")
    t_zt = Tok()
    pool.do(lambda: nc.gpsimd.memset(zt[:], 0.0), W=[t_zt])
    zview = h2s_d.rearrange("(n p r) d -> n p (r d)", p=128, r=2)
    t_zero = []
    for n in range(NROW // 256):
        tz = Tok()
        sp.dma(zview[n], zt[:], R=[t_zt], W=[tz])
        t_zero.append(tz)

    def rt(shape, dt=F32, name=None):
        return K.sb(shape, dt, name)
    t_r = Tok()
    gts = rt([128, 2, NTL]); desti = rt([128, 2, NTL], I32); idxG = rt([128, NSLOT, 8], I32); idxD = rt([128, NSLOT, 4], I32)
    hj = [K.sb([128, D], BF16, f"hj{i}") for i in range(2)]
    rmark = K.mark()
    gmx = rt([128, NTL]); ohg = rt([128, NTL, 4]); eg = rt([128, NTL, 4]); sg = rt([128, NTL]); pg = rt([128, NTL])
    pen = rt([128, NTL, 4]); me = rt([128, NTL, 32]); me2 = rt([128, NTL, 32]); oh1 = rt([128, NTL, 32]); oh2 = rt([128, NTL, 32])
    v1 = rt([128, NTL]); v2 = rt([128, NTL]); tmpw = rt([128, NTL, 32])
    OHb = rt([128, Wd], BF16); PCs = rt([128, NTL, 32]); Ts = rt([128, NTL, 32]); Rr = rt([128, NTL, 32])
    cnt = rt([128, 32]); cmp_ = rt([128, 32, 64]); nsl = rt([128, 32]); bsl = rt([128, 32]); incl = rt([128, 32]); base = rt([128, 32])
    destf = rt([128, 2, NTL]); cmp2 = rt([128, NSLOT, 32]); eslot = rt([128, NSLOT])
    idxf = rt([128, NSLOT, 8])
    gl = lg[:, :, 0:4]
    el = lg[:, :, 4:36]
    RED(gmx[:], gl, ALU.max, [t_lg], [t_r])
    TT(dve, ohg[:], gl, bc(gmx[:].unsqueeze(2), [128, NTL, 4]), ALU.is_equal, [t_lg, t_r], [t_r])
    TT(dve, eg[:], gl, bc(gmx[:].unsqueeze(2), [128, NTL, 4]), ALU.subtract, [t_lg, t_r], [t_r])
    ACT(eg[:], eg[:], AF.Exp, [t_r], [t_r])
    RED(sg[:], eg[:], ALU.add, [t_r], [t_r])
    dve.do(lambda: nc.vector.reciprocal(out=pg[:], in_=sg[:]), [t_r], [t_r])
    TS(dve, pen[:], ohg[:], -1.0, ALU.add, [t_r], [t_r], s2=1e30, op1=ALU.mult)
    TT(dve, me[:].rearrange("p j (g e) -> p j g e", g=4), el.rearrange("p j (g e) -> p j g e", g=4),
       bc(pen[:].unsqueeze(3), [128, NTL, 4, 8]), ALU.add, [t_lg, t_r], [t_r])
    RED(v1[:], me[:], ALU.max, [t_r], [t_r])
    TT(dve, oh1[:], me[:], bc(v1[:].unsqueeze(2), [128, NTL, 32]), ALU.is_equal, [t_r], [t_r])
    STT(me2[:].rearrange("p j e -> p (j e)"), oh1[:].rearrange("p j e -> p (j e)"), -1e30,
        me[:].rearrange("p j e -> p (j e)"), ALU.mult, ALU.add, [t_r], [t_r])
    RED(v2[:], me2[:], ALU.max, [t_r], [t_r])
    TT(dve, oh2[:], me2[:], bc(v2[:].unsqueeze(2), [128, NTL, 32]), ALU.is_equal, [t_r], [t_r])
    TT(dve, v2[:], v2[:], v1[:], ALU.subtract, [t_r], [t_r])
    ACT(v2[:], v2[:], AF.Exp, [t_r], [t_r])
    TS(dve, v2[:], v2[:], 1.0, ALU.add, [t_r], [t_r])
    dve.do(lambda: nc.vector.reciprocal(out=v2[:], in_=v2[:]), [t_r], [t_r])
    TT(dve, gts[:, 0, :], pg[:], v2[:], ALU.mult, [t_r], [t_r])
    TT(dve, gts[:, 1, :], pg[:], gts[:, 0, :], ALU.subtract, [t_r], [t_r])
    TT(dve, OHb[:], oh1[:].rearrange("p j e -> p (j e)"), oh2[:].rearrange("p j e -> p (j e)"), ALU.add, [t_r], [t_r])
    nchk = (Wd + 511) // 512
    for c in range(nchk):
        w_ = min(512, Wd - c * 512)
        MM(bank(c, w_), ustrb[:], OHb[:, c * 512:c * 512 + w_], True, True, [t_r, t_cb], [TB[c]])
        MM(bank(4 + c, w_), onesb[:], OHb[:, c * 512:c * 512 + w_], True, True, [t_r, t_cb], [TB[4 + c]])
    CP(dve, PCs[:].rearrange("p j e -> p (j e)"), PS[:, 0:Wd], [TB[c_] for c_ in range(nchk)], [t_r])
    CP(dve, Ts[:].rearrange("p j e -> p (j e)"), PS[:, 2048:2048 + Wd], [TB[4 + c_] for c_ in range(nchk)], [t_r])
    dve.do(lambda: nc.vector.memset(Rr[:, 0, :], 0.0), W=[t_r])
    for j in range(1, NTL):
        TT(dve, Rr[:, j, :], Rr[:, j - 1, :], Ts[:, j - 1, :], ALU.add, [t_r], [t_r])
    TT(dve, cnt[:], Rr[:, NTL - 1, :], Ts[:, NTL - 1, :], ALU.add, [t_r], [t_r])
    TT(dve, PCs[:], PCs[:], Rr[:], ALU.add, [t_r], [t_r])
    TT(dve, tmpw[:], oh1[:], PCs[:], ALU.mult, [t_r], [t_r])
    RED(destf[:, 0, :], tmpw[:], ALU.add, [t_r], [t_r])
    TT(dve, tmpw[:], oh2[:], PCs[:], ALU.mult, [t_r], [t_r])
    RED(destf[:, 1, :], tmpw[:], ALU.add, [t_r], [t_r])
    TT(dve, cmp_[:], bc(thr.unsqueeze(1), [128, 32, 64]), bc(cnt[:].unsqueeze(2), [128, 32, 64]), ALU.is_lt, [t_r, t_c], [t_r])
    RED(nsl[:], cmp_[:], ALU.add, [t_r], [t_r])
    dve.do(lambda: nc.vector.memset(bsl[:, 0:1], 0.0), W=[t_r])
    for e in range(1, 32):
        TT(dve, bsl[:, e:e + 1], bsl[:, e - 1:e], nsl[:, e - 1:e], ALU.add, [t_r], [t_r])
    TT(dve, incl[:], bsl[:], nsl[:], ALU.add, [t_r], [t_r])
    TS(dve, base[:], bsl[:], float(SL), ALU.mult, [t_r], [t_r])
    for k, ohk in ((0, oh1), (1, oh2)):
        TT(dve, tmpw[:], ohk[:], bc(base[:].unsqueeze(1), [128, NTL, 32]), ALU.mult, [t_r], [t_r])
        RED(v1[:], tmpw[:], ALU.add, [t_r], [t_r])
        TT(dve, destf[:, k, :], destf[:, k, :], v1[:], ALU.add, [t_r], [t_r])
    CP(dve, desti[:], destf[:], [t_r], [t_r])
    TT(dve, cmp2[:], bc(incl[:].unsqueeze(1), [128, NSLOT, 32]), bc(slotid[:, 0:NSLOT].unsqueeze(2), [128, NSLOT, 32]),
       ALU.is_le, [t_r, t_c], [t_r])
    RED(eslot[:], cmp2[:], ALU.add, [t_r], [t_r])
    TS(dve, eslot[:], eslot[:], 31.0, ALU.min, [t_r], [t_r])
    TS(dve, eslot[:], eslot[:], 128.0, ALU.mult, [t_r], [t_r], s2=rowoff[:, 0:1], op1=ALU.add)
    CP(dve, idxG[:, :, 0], eslot[:], [t_r], [t_r])
    K.barrier()
    K.release(rmark)

    t_scat = []
    t_hj = [Tok() for _ in range(2)]
    for j in range(NTL):
        sp.dma(hj[j % 2][:], h2_d[j * 128:(j + 1) * 128, :], W=[t_hj[j % 2]])
        for k in range(2):
            ts_ = Tok()
            pool.dma(h2s_d[:, :], hj[j % 2][:], R=[t_r, t_hj[j % 2]] + t_zero, W=[ts_],
                     indirect=dict(out_offset=IOA(ap=desti[:, k, j:j + 1], axis=0), in_offset=None))
            t_scat.append(ts_)

    wgb = [K.sb([128, 8, 512], BF16, f"wgb{i}") for i in range(2)]
    wub = [K.sb([128, 8, 512], BF16, f"wub{i}") for i in range(2)]
    wdb = [K.sb([128, 4, D], BF16, f"wdb{i}") for i in range(2)]
    t_w = [Tok() for _ in range(2)]
    hsb = [K.sb([128, 4, D], BF16, f"hs{i}") for i in range(2)]
    t_hsb = [Tok() for _ in range(2)]

    def load_hs(s):
        sp.dma(hsb[s % 2][:], h2s_d[s * SL:(s + 1) * SL, :].rearrange("(r p) d -> p r d", p=128), R=t_scat, W=[t_hsb[s % 2]])
    hT = K.sb([128, 8, 512], BF16, "hT")
    t_hT = Tok()
    sgb = [K.sb([128, 512], BF16, f"sgb{i}") for i in range(2)]
    t_sgb = [Tok() for _ in range(2)]
    aT = K.sb([128, 4, 512], BF16, "aT")
    t_aT = [Tok() for _ in range(4)]
    yst = [K.sb([128, D], F32, f"yst{i}") for i in range(2)]
    t_yst = [Tok() for _ in range(2)]
    t_ys = []

    def load_w(s):
        q = s % 2
        pool.dma(wgb[q][:].rearrange("p k f -> p (k f)"), wg_d, R=[t_r], W=[t_w[q]],
                 indirect=dict(out_offset=None, in_offset=IOA(ap=idxG[:, s, 0:1], axis=0)))
        pool.dma(wub[q][:].rearrange("p k f -> p (k f)"), wu_d, R=[t_r], W=[t_w[q]],
                 indirect=dict(out_offset=None, in_offset=IOA(ap=idxG[:, s, 0:1], axis=0)))
        pool.dma(wdb[q][:].rearrange("p k f -> p (k f)"), wd_d, R=[t_r], W=[t_w[q]],
                 indirect=dict(out_offset=None, in_offset=IOA(ap=idxG[:, s, 0:1], axis=0)))

    load_w(0)
    load_hs(0)
    yq = 0
    for s in range(NSLOT):
        q = s % 2
        hs = hsb[q]
        t_hs = t_hsb[q]
        if s + 1 < NSLOT:
            load_w(s + 1)
            load_hs(s + 1)
        for r in range(4):
            b0 = 0 if r % 2 == 0 else 7
            for kc in range(8):
                TR(bankbf(b0)[:, kc * 128:(kc + 1) * 128], hs[:, r, kc * 128:(kc + 1) * 128], identb[:], [t_hs, t_cb], [TB[b0]])
            TT(dve, hT[:, :, r * 128:(r + 1) * 128], bankbf(b0).rearrange("p (k c) -> p k c", k=8),
               bc(n2w[:].unsqueeze(2), [128, 8, 128]), ALU.mult, [TB[b0], t_par], [t_hT])
        for fc in range(4):
            bg = [1, 3][fc % 2]
            bu = [2, 6][fc % 2]
            for kc in range(8):
                MM(bank(bg), wgb[q][:, kc, fc * 128:(fc + 1) * 128], hT[:, kc, :], kc == 0, kc == 7, [t_w[q], t_hT], [TB[bg]])
            for kc in range(8):
                MM(bank(bu), wub[q][:, kc, fc * 128:(fc + 1) * 128], hT[:, kc, :], kc == 0, kc == 7, [t_w[q], t_hT], [TB[bu]])
            ACT(sgb[fc % 2][:], bank(bg), AF.Silu, [TB[bg]], [t_sgb[fc % 2]])
            TT(dve, aT[:, fc, :], sgb[fc % 2][:], bank(bu), ALU.mult, [t_sgb[fc % 2], TB[bu]], [t_aT[fc]])
        for r in range(4):
            db = 4 if r % 2 == 0 else 6
            for nh in range(2):
                for fc in range(4):
                    MM(bank(db + nh), aT[:, fc, r * 128:(r + 1) * 128], wdb[q][:, fc, nh * 512:(nh + 1) * 512], fc == 0, fc == 3,
                       [t_aT[fc], t_w[q]], [TB[db + nh]])
            CP(act, yst[yq][:], PS[:, db * 512:(db + 2) * 512], [TB[db], TB[db + 1]], [t_yst[yq]])
            ty = Tok()
            sp.dma(ys_d[s * SL + r * 128: s * SL + (r + 1) * 128, :], yst[yq][:], R=[t_yst[yq]], W=[ty])
            t_ys.append(ty)
            yq ^= 1

    NB2 = 2
    ya = [K.sb([128, D], F32, f"ya{i}") for i in range(NB2)]
    yb = [K.sb([128, D], F32, f"yb{i}") for i in range(NB2)]
    x1t = [K.sb([128, D], F32, f"x1t{i}") for i in range(NB2)]
    acc = [K.sb([128, D], F32, f"acc{i}") for i in range(NB2)]
    ot = [K.sb([128, D], F32, f"ot{i}") for i in range(NB2)]
    jk = K.sb([128, D], BF16, "jk")
    s5 = K.sb([128, 4 * NB2], F32, "s5")
    t_ya = [Tok() for _ in range(NB2)]
    t_yb = [Tok() for _ in range(NB2)]
    t_x1t = [Tok() for _ in range(NB2)]
    t_acc = [Tok() for _ in range(NB2)]
    t_ot = [Tok() for _ in range(NB2)]
    t_s5 = [Tok() for _ in range(NB2)]
    t_jk = Tok()
    t_out = []
    for j in range(NTL):
        u = j % NB2
        c0 = 4 * u
        pool.dma(ya[u][:], ys_d, R=[t_r] + t_ys, W=[t_ya[u]],
                 indirect=dict(out_offset=None, in_offset=IOA(ap=desti[:, 0, j:j + 1], axis=0)))
        pool.dma(yb[u][:], ys_d, R=[t_r] + t_ys, W=[t_yb[u]],
                 indirect=dict(out_offset=None, in_offset=IOA(ap=desti[:, 1, j:j + 1], axis=0)))
        sp.dma(x1t[u][:], x1_d[j * 128:(j + 1) * 128, :], W=[t_x1t[u]])
        STT(acc[u][:], ya[u][:], gts[:, 0, j:j + 1], x1t[u][:], ALU.mult, ALU.add, [t_ya[u], t_x1t[u], t_r], [t_acc[u]])
        STT(acc[u][:], yb[u][:], gts[:, 1, j:j + 1], acc[u][:], ALU.mult, ALU.add, [t_yb[u], t_acc[u], t_r], [t_acc[u]])
        ACT(jk[:], acc[u][:], AF.Square, [t_acc[u]], [t_jk, t_s5[u]], accum_out=s5[:, c0:c0 + 1])
        ACT(s5[:, c0 + 1:c0 + 2], s5[:, c0:c0 + 1], AF.Ln, [t_s5[u]], [t_s5[u]], scale=1.0 / D, bias=EPS)
        ACT(s5[:, c0 + 2:c0 + 3], s5[:, c0 + 1:c0 + 2], AF.Exp, [t_s5[u]], [t_s5[u]], scale=-0.5)
        STT(ot[u][:], acc[u][:], s5[:, c0 + 2:c0 + 3], nfw[:], ALU.mult, ALU.mult, [t_acc[u], t_s5[u], t_nfw], [t_ot[u]])
        to = Tok()
        sp.dma(out_d[j * 128:(j + 1) * 128, :], ot[u][:], R=[t_ot[u]], W=[to])
        t_out.append(to)
    K.barrier()
    return K


def _shared_inputs(inp):
    f = lambda a: np.ascontiguousarray(np.asarray(a, dtype=np.float32))
    w_in = f(inp["w_in"])[0]
    order = np.concatenate([np.arange(1024, 2560), np.arange(0, 1024), np.arange(2560, 2576),
                            np.arange(6672, 6688), np.arange(2576, 4624), np.arange(4624, 5648),
                            np.arange(5648, 6672)])
    w_in_p = w_in[:, order].reshape(8, 128, NCOLS).transpose(1, 0, 2)
    w_out = f(inp["w_out"])[0].reshape(16, 128, D).transpose(1, 0, 2)
    cwf = np.concatenate([f(inp["ssd_conv_w"])[0], f(inp["ml_conv_w"])[0]], axis=1)
    cw = cwf.T.reshape(28, 128, 4).transpose(1, 0, 2)
    cbf = np.concatenate([f(inp["ssd_conv_b"])[0], f(inp["ml_conv_b"])[0]])
    cb = cbf.reshape(28, 128).T
    n1w = f(inp["norm1_w"])[0].reshape(8, 128).T
    n2w = f(inp["norm2_w"])[0].reshape(8, 128).T
    repv = np.concatenate([f(inp["ssd_dt_bias"])[0], f(inp["ssd_a_log"])[0], f(inp["ssd_d"])[0],
                           f(inp["ml_i_bias"])[0], f(inp["ml_f_bias"])[0],
                           f(inp["router_g_b"])[0], f(inp["router_e_b"])[0]])
    rep = np.tile(repv[None, :], (128, 1))
    snw = np.tile(f(inp["ssd_norm_w"])[0][None, :], (128, 1))
    mnw = np.tile(f(inp["ml_norm_w"])[0][None, :], (128, 1))
    nfw = np.tile(f(inp["norm_f_w"])[None, :], (128, 1))
    wr = np.concatenate([f(inp["router_g_w"])[0], f(inp["router_e_w"])[0]], axis=1).reshape(8, 128, 36).transpose(1, 0, 2)
    s = np.arange(128)
    ident = np.eye(128, dtype=np.float32)
    tri = (s[:, None] <= s[None, :]).astype(np.float32)
    ones = np.ones((128, 128), np.float32)
    ustr = (s[:, None] < s[None, :]).astype(np.float32)
    sel = np.zeros((128, 8, 128), np.float32)
    for h in range(8):
        sel[h, h, :] = 1.0
    thr = np.tile((np.arange(64, dtype=np.float32) * SL)[None, :], (128, 1))
    slotid = np.tile(np.arange(128, dtype=np.float32)[None, :], (128, 1))
    rowoff = (np.arange(24, dtype=np.float32)[None, :] * 128 + s[:, None]).astype(np.float32)
    cst = np.concatenate([ident, tri, ones, ustr, sel.reshape(128, 1024), thr, slotid, rowoff], axis=1)
    d = dict(w_in=w_in_p, w_out=w_out, cw=cw, cb=cb, n1w=n1w, n2w=n2w, rep=rep, snw=snw, mnw=mnw, nfw=nfw,
             wr=wr, cst=cst,
             wg=f(inp["exp_w_gate"])[0].reshape(NE, 8, 128, 512).transpose(0, 2, 1, 3).reshape(NE * 128, 8 * 512),
             wu=f(inp["exp_w_up"])[0].reshape(NE, 8, 128, 512).transpose(0, 2, 1, 3).reshape(NE * 128, 8 * 512),
             wd=f(inp["exp_w_down"])[0].reshape(NE, 4, 128, D).transpose(0, 2, 1, 3).reshape(NE * 128, 4 * D))
    return {k: np.ascontiguousarray(v, dtype=np.float32) for k, v in d.items()}


def kernel(**inputs):
    x = np.asarray(inputs["x"], dtype=np.float32)
    B, L, _ = x.shape
    ncores = 8
    nseq = B // ncores
    shared = _shared_inputs(inputs)
    K = build(nseq, L)
    in_maps = []
    for c in range(ncores):
        m = dict(shared)
        m["x"] = np.ascontiguousarray(x[c * nseq:(c + 1) * nseq].reshape(nseq * L, D))
        in_maps.append(m)
    res = run_bass_kernel_spmd(K.nc, in_maps, core_ids=list(range(ncores)))
    outs = [np.asarray(r["out"], dtype=np.float32).reshape(nseq, L, D) for r in res.results]
    return np.concatenate(outs, axis=0)
```

```python
import numpy as np
import concourse.bass as bass
import concourse.mybir as mybir
from concourse.bass_utils import run_bass_kernel_spmd

F32 = mybir.dt.float32
BF16 = mybir.dt.bfloat16
I32 = mybir.dt.int32
AF = mybir.ActivationFunctionType
ALU = mybir.AluOpType
AX = mybir.AxisListType

D = 1024
NCOLS = 6688
EPS = 1e-6
NEG = -30000.0
SL = 512
NE = 32


LOG = []


class Tok:
    __slots__ = ("w", "r", "excl")

    def __init__(self, excl=False):
        self.w = None
        self.r = []
        self.excl = excl


class Chan:
    def __init__(self, nc, name):
        self.sem = nc.alloc_semaphore(name=name)
        self.cnt = 0

    def value_for(self, seq):
        return self.sem, seq


class Eng:
    def __init__(self, nc, name, h, raw_safe=False):
        self.nc = nc
        self.name = name
        self.h = h
        self.sem = nc.alloc_semaphore(name="se_" + name)
        self.cnt = 0
        self.seq = 0
        self.last = None
        self.incs = []
        self.seen = {}
        self.raw_safe = raw_safe
        self.chans = []
        self.chan_i = 0
        self.nwaits = 0
        self.ninst = 0

    def value_for(self, seq):
        best = None
        for s, v in reversed(self.incs):
            if s >= seq:
                best = v
            else:
                break
        if best is not None:
            return self.sem, best
        assert self.last is not None and self.seq >= seq
        self.last.then_inc(self.sem, 1)
        self.cnt += 1
        LOG.append((self.name, "inc", self.seq, self.cnt))
        self.incs.append((self.seq, self.cnt))
        if len(self.incs) > 256:
            self.incs = self.incs[-128:]
        return self.sem, self.cnt

    def wait_on(self, dep):
        src, seq = dep
        if src is self and self.raw_safe:
            return
        sem, val = src.value_for(seq)
        if self.seen.get(sem, 0) >= val:
            return
        self.h.wait_ge(sem, val)
        LOG.append((self.name, "wait", str(sem), val))
        self.nwaits += 1
        self.seen[sem] = val

    def _deps(self, R, W):
        for t in R:
            if t.w is not None:
                self.wait_on(t.w)
            if t.excl:
                for d in t.r:
                    if d[0] is not self:
                        self.wait_on(d)
        for t in W:
            if t.w is not None and t.w[0] is not self:
                self.wait_on(t.w)
            for d in t.r:
                if d[0] is not self:
                    self.wait_on(d)

    def do(self, fn, R=(), W=()):
        self._deps(R, W)
        inst = fn()
        self.seq += 1
        self.ninst += 1
        self.last = inst
        LOG.append((self.name, "inst", self.seq, type(inst).__name__))
        me = (self, self.seq)
        for t in R:
            t.r.append(me)
        for t in W:
            t.w = me
            t.r = []
        return inst

    def dma(self, out, in_, R=(), W=(), indirect=None):
        ch = self.chans[self.chan_i]
        self.chan_i = (self.chan_i + 1) % len(self.chans)
        if ch.cnt and self.seen.get(ch.sem, 0) < ch.cnt:
            self.h.wait_ge(ch.sem, ch.cnt)
            self.seen[ch.sem] = ch.cnt
        self._deps(R, W)
        if indirect is None:
            inst = self.h.dma_start(out=out, in_=in_)
        else:
            inst = self.h.indirect_dma_start(out=out, in_=in_, **indirect)
        inst.then_inc(ch.sem, 16)
        ch.cnt += 16
        self.ninst += 1
        me = (ch, ch.cnt)
        for t in R:
            t.r.append(me)
        for t in W:
            t.w = me
            t.r = []
        return inst


class KB:
    def __init__(self):
        self.nc = bass.Bass("TRN2", target_bir_lowering=False)
        nc = self.nc
        self.pe = Eng(nc, "pe", nc.tensor, raw_safe=True)
        self.act = Eng(nc, "act", nc.scalar)
        self.dve = Eng(nc, "dve", nc.vector)
        self.pool = Eng(nc, "pool", nc.gpsimd)
        self.sp = Eng(nc, "sp", nc.sync)
        self.sp.chans = [Chan(nc, f"c_sp{i}") for i in range(8)]
        self.pool.chans = [Chan(nc, f"c_pl{i}") for i in range(8)]
        self.engs = [self.pe, self.act, self.dve, self.pool, self.sp]
        self._stack = []
        self._names = 0

    def sb(self, shape, dt, name=None):
        self._names += 1
        g = self.nc.sbuf_tensor(f"s{self._names}_{name or 'sb'}", list(shape), dt)
        t = g.__enter__()
        self._stack.append(g)
        return t

    def ps(self, shape, dt, name=None):
        self._names += 1
        g = self.nc.psum_tensor(f"p{self._names}_{name or 'ps'}", list(shape), dt)
        t = g.__enter__()
        self._stack.append(g)
        return t

    def barrier(self):
        LOG.append(("all", "barrier", 0, 0))
        for e in self.engs:
            for o in self.engs:
                if o is not e and o.seq > 0:
                    e.wait_on((o, o.seq))
            for o in self.engs:
                for ch in o.chans:
                    if ch.cnt:
                        e.wait_on((ch, ch.cnt))

    def mark(self):
        return len(self._stack)

    def release(self, mark):
        while len(self._stack) > mark:
            g = self._stack.pop()
            g.__exit__(None, None, None)


class _Cut(Exception):
    pass


def build(NSEQ, L, stop_after=None, cut=None):
    try:
        return _build(NSEQ, L, stop_after, cut)
    except _Cut as e:
        K = e.args[0]
        K.barrier()
        return K


def _build(NSEQ, L, stop_after=None, cut=None):
    K = KB()

    def CUT(n):
        if cut == n:
            raise _Cut(K)

    nc = K.nc
    pe, act, dve, pool, sp = K.pe, K.act, K.dve, K.pool, K.sp
    NT = NSEQ * L
    NTILE = NT // 128
    NBLK = L // 512
    NSLOT = (2 * NT) // SL + NE
    NROW = NSLOT * SL

    def din(name, shape, dt=F32):
        return nc.dram_tensor(name, list(shape), dt, kind="ExternalInput").ap()

    def dscr(name, shape, dt):
        return nc.dram_tensor(name, list(shape), dt, kind="Internal").ap()

    x_d = din("x", [NT, D])
    win_d = din("w_in", [128, 8, NCOLS])
    wout_d = din("w_out", [128, 16, D])
    cw_d = din("cw", [128, 28, 4])
    cb_d = din("cb", [128, 28])
    n1w_d = din("n1w", [128, 8])
    n2w_d = din("n2w", [128, 8])
    rep_d = din("rep", [128, 16 * 3 + 8 * 2 + 36])
    snw_d = din("snw", [128, D])
    mnw_d = din("mnw", [128, D])
    nfw_d = din("nfw", [128, D])
    wr_d = din("wr", [128, 8, 36])
    cst_d = din("cst", [128, 128 * 4 + 1024 + 64 + 128 + 24])
    wg_d = din("wg", [NE * 128, 8 * 512])
    wu_d = din("wu", [NE * 128, 8 * 512])
    wd_d = din("wd", [NE * 128, 4 * D])
    out_d = nc.dram_tensor("out", [NT, D], F32, kind="ExternalOutput").ap()
    x1_d = nc.dram_tensor("x1s", [NT, D], F32, kind=("ExternalOutput" if stop_after else "Internal")).ap()
    lg_d = nc.dram_tensor("lgs", [NT, 36], F32, kind=("ExternalOutput" if stop_after else "Internal")).ap()
    h2_d = dscr("h2s", [NT, D], BF16)
    h2s_d = dscr("h2sorted", [NROW, D], BF16)
    ys_d = dscr("yss", [NROW, D], F32)

    PS = K.ps([128, 4096], F32, "psall")
    TB = [Tok(excl=True) for _ in range(8)]

    def bank(b, n=512, p=128):
        return PS[0:p, b * 512:b * 512 + n]

    def bankbf(b):
        return PS[:, b * 512:(b + 1) * 512].bitcast(BF16)

    def ACT(out, in_, func, R, W, **kw):
        return act.do(lambda: nc.scalar.activation(out=out, in_=in_, func=func, **kw), R, W)

    def TT(e, out, a, b, op, R, W):
        return e.do(lambda: e.h.tensor_tensor(out=out, in0=a, in1=b, op=op), R, W)

    def TS(e, out, a, s1, op0, R, W, s2=None, op1=None):
        if op1 is None:
            return e.do(lambda: e.h.tensor_scalar(out=out, in0=a, scalar1=s1, scalar2=None, op0=op0), R, W)
        return e.do(lambda: e.h.tensor_scalar(out=out, in0=a, scalar1=s1, scalar2=s2, op0=op0, op1=op1), R, W)

    def STT(out, a, s, b, op0, op1, R, W):
        return dve.do(lambda: nc.vector.scalar_tensor_tensor(out=out, in0=a, scalar=s, in1=b, op0=op0, op1=op1), R, W)

    def CP(e, out, in_, R, W):
        if e is act:
            return act.do(lambda: nc.scalar.copy(out=out, in_=in_), R, W)
        return e.do(lambda: e.h.tensor_copy(out=out, in_=in_), R, W)

    def MM(out, lhsT, rhs, start, stop, R, W):
        return pe.do(lambda: nc.tensor.matmul(out, lhsT, rhs, start=start, stop=stop), R, W)

    def TR(out, in_, ident, R, W):
        return pe.do(lambda: nc.tensor.transpose(out, in_, ident), R, W)

    def RED(out, in_, op, R, W, axis=AX.X):
        return dve.do(lambda: nc.vector.tensor_reduce(out=out, in_=in_, axis=axis, op=op), R, W)

    def bc(ap, shape):
        return ap.broadcast_to(list(shape))

    cst = K.sb([128, 128 * 4 + 1024 + 64 + 128 + 24], F32, "cst")
    t_c = Tok()
    sp.dma(cst[:], cst_d, W=[t_c])
    identf = cst[:, 0:128]
    trif = cst[:, 128:256]
    onesf = cst[:, 256:384]
    ustr = cst[:, 384:512]
    sel8 = cst[0:8, 512:1536]
    thr = cst[:, 1536:1600]
    slotid = cst[:, 1600:1728]
    rowoff = cst[:, 1728:1752]
    identb = K.sb([128, 128], BF16, "identb")
    onesb = K.sb([128, 128], BF16, "onesb")
    ustrb = K.sb([128, 128], BF16, "ustrb")
    maskb = K.sb([128, 8, 128], BF16, "maskb")
    t_cb = Tok()
    CP(dve, identb[:], identf, [t_c], [t_cb])
    CP(dve, onesb[:], onesf, [t_c], [t_cb])
    CP(dve, ustrb[:], ustr, [t_c], [t_cb])
    for h in range(8):
        TS(dve, maskb[:, h, :], trif, -1.0, ALU.add, [t_c], [t_cb], s2=-NEG, op1=ALU.mult)

    rep = K.sb([128, 100], F32, "rep")
    t_rep = Tok()
    sp.dma(rep[:], rep_d, W=[t_rep])
    dtb_b = rep[:, 0:16]
    alog_b = rep[:, 16:32]
    dsk_b = rep[:, 32:48]
    ib_b = rep[:, 48:56]
    fb_b = rep[:, 56:64]
    rb_b = rep[:, 64:100]
    a_b = K.sb([128, 16], F32, "a_b")
    t_ab = Tok()
    ACT(a_b[:], alog_b, AF.Exp, [t_rep], [t_ab])
    TS(dve, a_b[:], a_b[:], -1.0, ALU.mult, [t_ab], [t_ab])
    n1w = K.sb([128, 8], F32, "n1w")
    n2w = K.sb([128, 8], F32, "n2w")
    cw = K.sb([128, 28, 4], F32, "cw")
    cbias = K.sb([128, 28], F32, "cbias")
    t_par = Tok()
    sp.dma(n1w[:], n1w_d, W=[t_par])
    sp.dma(n2w[:], n2w_d, W=[t_par])
    sp.dma(cw[:], cw_d, W=[t_par])
    sp.dma(cbias[:], cb_d, W=[t_par])
    wr = K.sb([128, 8, 36], F32, "wr")
    sp.dma(wr[:], wr_d, W=[t_par])
    wsmf = K.sb([128, 8, 32], F32, "wsmf")
    wsm = K.sb([128, 8, 32], BF16, "wsm")
    t_wsm = Tok()
    sp.dma(wsmf[:], win_d[:, :, 2560:2592], W=[t_wsm])
    CP(dve, wsm[:], wsmf[:], [t_wsm], [t_wsm])

    CUT(0)
    ph1 = K.mark()
    snw = K.sb([128, D], BF16, "snw")
    mnw = K.sb([128, D], BF16, "mnw")
    t_nw = Tok()
    pool.dma(snw[:], snw_d, W=[t_nw])
    pool.dma(mnw[:], mnw_d, W=[t_nw])
    dgp = [K.sb([128, 4, 4, 128], BF16, f"dgp{i}") for i in range(2)]
    t_dgp = [Tok() for _ in range(2)]
    t_dg = Tok()
    dpar = [0]
    dgD = K.sb([128, 16, 128], BF16, "dgD")
    for h in range(16):
        TS(dve, dgD[:, h, :], identf, dsk_b[:, h:h + 1], ALU.mult, [t_c, t_rep], [t_dg])
    wout = K.sb([128, 16, D], BF16, "wout")
    t_wout = Tok()
    for e2 in range(4):
        pool.dma(wout[:, e2 * 4:(e2 + 1) * 4, :], wout_d[:, e2 * 4:(e2 + 1) * 4, :], W=[t_wout])

    NWB = 2
    wbuf = [K.sb([128, 8, 512], BF16, f"wbuf{i}") for i in range(NWB)]
    t_wbuf = [Tok() for _ in range(NWB)]
    xt = K.sb([128, D], F32, "xt")
    t_xt = Tok()
    junk = K.sb([128, D], BF16, "junk")
    t_junk = Tok()
    xn = K.sb([128, D], BF16, "xn")
    t_xn = Tok()
    st4 = K.sb([128, 16], F32, "st4")
    t_st4 = Tok()
    hnT = K.sb([128, 8, 512], BF16, "hnT")
    t_hnT = Tok()
    pb = [K.sb([128, 515], BF16, f"pb{i}") for i in range(2)]
    t_pb = [Tok() for _ in range(2)]
    hal = K.sb([128, 28, 4], BF16, "hal")
    t_hal = [Tok() for _ in range(28)]
    cvS = K.sb([128, 12, 512], BF16, "cvS")
    t_cvS = Tok()
    cvM = K.sb([128, 16, 512], BF16, "cvM")
    t_cvM = Tok()
    zs = K.sb([128, 4, D], BF16, "zs")
    t_zs = Tok()
    vtm = K.sb([128, 4, D], BF16, "vtm")
    t_v = Tok()
    so = K.sb([128, 4, D], BF16, "so")
    t_so = Tok()
    gates = K.sb([128, 4, 32], F32, "gates")
    t_g = Tok()
    gs = K.sb([128, 4, 16 * 2 + 8 * 2], F32, "gs")
    t_gs = Tok()
    sm = K.sb([128, 160], F32, "sm")
    t_sm = Tok()
    csT = K.sb([8, 4, 128], F32, "csT")
    t_csT = Tok()
    dve.do(lambda: nc.vector.memset(csT[:, 3, :], -1.0), W=[t_csT])
    bd = K.sb([8, 8, 128], F32, "bd")
    t_bd = Tok()
    Lt = K.sb([128, 8, 128], BF16, "Lt")
    t_Lt = Tok()
    Mt = K.sb([128, 8, 128], BF16, "Mt")
    t_Mt = Tok()
    cbt = K.sb([128, 128], BF16, "cbt")
    t_cbt = Tok()
    xs_tm = K.sb([128, D], BF16, "xs_tm")
    t_xs = Tok()
    xdt_tm = K.sb([128, D], BF16, "xdt_tm")
    t_xdt = Tok()
    xdec = K.sb([128, D], BF16, "xdec")
    t_xdec = Tok()
    B_tm = K.sb([128, 256], BF16, "B_tm")
    t_Btm = Tok()
    k_tm = K.sb([128, D], BF16, "k_tm")
    t_ktm = Tok()
    kk = K.sb([128, D], BF16, "kk")
    t_kk = Tok()
    fa = K.sb([128, D], F32, "fa")
    t_fa = Tok()
    fb = K.sb([128, D], F32, "fb")
    t_fb = Tok()
    ynb = K.sb([128, D], BF16, "ynb")
    t_ynb = Tok()
    mixT = K.sb([128, 16, 128], BF16, "mixT")
    t_mixT = Tok()
    stS = K.sb([128, D], F32, "stS")
    t_stS = Tok()
    stSb = K.sb([128, D], BF16, "stSb")
    t_stSb = Tok()
    Cm = K.sb([128, D], F32, "Cm")
    t_Cm = Tok()
    Cmb = K.sb([128, D], BF16, "Cmb")
    t_Cmb = Tok()
    nm = K.sb([128, 8], F32, "nm")
    t_nm = Tok()
    nmb = K.sb([128, 8], BF16, "nmb")
    t_nmb = Tok()
    xhb = K.sb([128, D], BF16, "xhb")
    t_xhb = Tok()
    h2T = K.sb([128, 8, 128], F32, "h2T")
    t_h2T = Tok()
    lgt = K.sb([128, 36], F32, "lgt")
    t_lgt = Tok()

    pieces = []
    for i in range(3):
        pieces.append(("feat", i * 512, 512, ("S", i * 4)))
    for i in range(2):
        pieces.append(("tok", 1536 + i * 512, 512, ("z", i)))
    pieces.append(("gate", 2560, 32, ("g", 0)))
    for i in range(4):
        pieces.append(("feat", 2592 + i * 512, 512, ("M", i * 4)))
    for i in range(2):
        pieces.append(("tok", 4640 + i * 512, 512, ("v", i)))
    for i in range(2):
        pieces.append(("tok", 5664 + i * 512, 512, ("o", i)))
    NP = len(pieces)
    wq = []
    piece_ctr = [0]

    def issue_piece_load(gi):
        kind, c0, ncl, arg = pieces[gi % NP]
        slot = gi % NWB
        if kind == "gate":
            return
        pool.dma(wbuf[slot][:, :, 0:ncl], win_d[:, :, c0:c0 + ncl], W=[t_wbuf[slot]])

    total_pieces = NSEQ * NBLK * NP
    nxt_load = [0]

    def ensure_loaded(gi):
        while nxt_load[0] < min(total_pieces, gi + NWB):
            issue_piece_load(nxt_load[0])
            nxt_load[0] += 1

    ppar = [0]
    cpar = [0]
    cvpar = [0]

    for seq in range(NSEQ):
        dve.do(lambda: nc.vector.memset(stS[:], 0.0), W=[t_stS])
        dve.do(lambda: nc.vector.memset(stSb[:], 0.0), W=[t_stSb])
        dve.do(lambda: nc.vector.memset(Cm[:], 0.0), W=[t_Cm])
        dve.do(lambda: nc.vector.memset(Cmb[:], 0.0), W=[t_Cmb])
        dve.do(lambda: nc.vector.memset(nm[:], 0.0), W=[t_nm])
        dve.do(lambda: nc.vector.memset(nmb[:], 0.0), W=[t_nmb])
        for ci in range(28):
            pool.do(lambda ci=ci: nc.gpsimd.memset(hal[:, ci, :], 0.0), W=[t_hal[ci]])
        for blk in range(NBLK):
            tok0 = seq * L + blk * 512
            gbase = (seq * NBLK + blk) * NP
            ensure_loaded(gbase)
            for t in range(4):
                sp.dma(xt[:], x_d[tok0 + t * 128: tok0 + (t + 1) * 128, :], W=[t_xt])
                ACT(junk[:], xt[:], AF.Square, [t_xt], [t_junk, t_st4], accum_out=st4[:, 0:1])
                ACT(st4[:, 1:2], st4[:, 0:1], AF.Ln, [t_st4], [t_st4], scale=1.0 / D, bias=EPS)
                ACT(st4[:, 2:3], st4[:, 1:2], AF.Exp, [t_st4], [t_st4], scale=-0.5)
                TS(dve, xn[:], xt[:], st4[:, 2:3], ALU.mult, [t_xt, t_st4], [t_xn])
                b0 = 0 if t % 2 == 0 else 7
                for kc in range(8):
                    TR(bankbf(b0)[:, kc * 128:(kc + 1) * 128], xn[:, kc * 128:(kc + 1) * 128], identb[:],
                       [t_xn, t_cb], [TB[b0]])
                TT(dve, hnT[:, :, t * 128:(t + 1) * 128],
                   bankbf(b0).rearrange("p (k c) -> p k c", k=8),
                   bc(n1w[:].unsqueeze(2), [128, 8, 128]), ALU.mult, [TB[b0], t_par], [t_hnT])
            CUT(1)
            for pi in range(NP):
                gi = gbase + pi
                ensure_loaded(gi)
                kind, c0, ncl, arg = pieces[pi]
                slot = gi % NWB
                wb = wbuf[slot]
                tw = t_wbuf[slot]
                if kind == "gate":
                    wb = wsm
                    tw = t_wsm
                    kind = "tok"
                if kind == "feat":
                    which, ci0 = arg
                    dq = dpar[0]
                    dpar[0] ^= 1
                    for cc in range(4):
                        gci_ = (ci0 + cc) if which == "S" else 12 + ci0 + cc
                        for k in range(4):
                            TS(dve, dgp[dq][:, cc, k, :], identf, cw[:, gci_, k:k + 1], ALU.mult, [t_c, t_par], [t_dgp[dq]])
                    IPB = [1, 2, 4, 5]
                    CVB = [3, 6]

                    def inproj(cc):
                        ci = ci0 + cc
                        gci = ci if which == "S" else 12 + ci
                        b = IPB[ppar[0] % 4]
                        ppar[0] += 1
                        for kc in range(8):
                            MM(bank(b), wb[:, kc, cc * 128:(cc + 1) * 128], hnT[:, kc, :], kc == 0, kc == 7,
                               [tw, t_hnT], [TB[b]])
                        q = cpar[0] % 2
                        cpar[0] += 1
                        CP(pool, pb[q][:, 0:3], hal[:, gci, 0:3], [t_hal[gci]], [t_pb[q]])
                        CP(act, pb[q][:, 3:515], bank(b), [TB[b]], [t_pb[q]])
                        CP(pool, hal[:, gci, 0:3], pb[q][:, 512:515], [t_pb[q]], [t_hal[gci]])
                        return q

                    def conv(cc, q):
                        ci = ci0 + cc
                        gci = ci if which == "S" else 12 + ci
                        cb_ = CVB[cvpar[0] % 2]
                        cvpar[0] += 1
                        for k in range(4):
                            MM(bank(cb_), dgp[dq][:, cc, k, :], pb[q][:, k:k + 512], k == 0, k == 3,
                               [t_dgp[dq], t_pb[q]], [TB[cb_]])
                        if which == "S":
                            ACT(cvS[:, ci, :], bank(cb_), AF.Silu, [TB[cb_], t_par], [t_cvS], bias=cbias[:, gci:gci + 1])
                        else:
                            ACT(cvM[:, ci, :], bank(cb_), AF.Silu, [TB[cb_], t_par], [t_cvM], bias=cbias[:, gci:gci + 1])

                    qprev = inproj(0)
                    for cc in range(1, 4):
                        qn = inproj(cc)
                        conv(cc - 1, qprev)
                        qprev = qn
                    conv(3, qprev)
                else:
                    which, half = arg
                    for t in range(4):
                        b = [1, 2, 4, 5][ppar[0] % 4]
                        ppar[0] += 1
                        for kc in range(8):
                            MM(bank(b, ncl), hnT[:, kc, t * 128:(t + 1) * 128], wb[:, kc, 0:ncl], kc == 0, kc == 7,
                               [tw, t_hnT], [TB[b]])
                        if which == "z":
                            ACT(zs[:, t, half * 512:(half + 1) * 512], bank(b), AF.Silu, [TB[b]], [t_zs])
                        elif which == "v":
                            CP(act, vtm[:, t, half * 512:(half + 1) * 512], bank(b), [TB[b]], [t_v])
                        elif which == "o":
                            ACT(so[:, t, half * 512:(half + 1) * 512], bank(b), AF.Sigmoid, [TB[b]], [t_so])
                        else:
                            CP(dve, gates[:, t, :], bank(b, 32), [TB[b]], [t_g])
                CUT(10 + pi)
            CUT(2)
            dtv = gs[:, :, 0:16]
            adt = gs[:, :, 16:32]
            spv = gs[:, :, 32:40]
            ipv = gs[:, :, 40:48]
            TT(dve, dtv, gates[:, :, 0:16], bc(dtb_b.unsqueeze(1), [128, 4, 16]), ALU.add, [t_g, t_rep], [t_gs])
            ACT(dtv, dtv, AF.Exp, [t_gs], [t_gs])
            ACT(dtv, dtv, AF.Ln, [t_gs], [t_gs], bias=1.0)
            TT(dve, adt, dtv, bc(a_b[:].unsqueeze(1), [128, 4, 16]), ALU.mult, [t_gs, t_ab], [t_gs])
            TT(dve, spv, gates[:, :, 24:32], bc(fb_b.unsqueeze(1), [128, 4, 8]), ALU.add, [t_g, t_rep], [t_gs])
            ACT(spv, spv, AF.Exp, [t_gs], [t_gs], scale=-1.0)
            ACT(spv, spv, AF.Ln, [t_gs], [t_gs], bias=1.0)
            TT(dve, ipv, gates[:, :, 16:24], bc(ib_b.unsqueeze(1), [128, 4, 8]), ALU.add, [t_g, t_rep], [t_gs])
            TS(dve, ipv, ipv, float(np.log(128.0 ** -0.5)), ALU.add, [t_gs], [t_gs])

            CUT(3)
            import os as _os
            for t in range(int(_os.environ.get("KT0", "0")), 4):
                tc_ = slice(t * 128, (t + 1) * 128)
                for cc in range(8):
                    TR(bankbf(0)[:, cc * 128:(cc + 1) * 128], cvS[:, cc, tc_], identb[:], [t_cvS, t_cb], [TB[0]])
                CUT(200 + 10 * t + 0)
                CP(act, xs_tm[:], bankbf(0), [TB[0]], [t_xs])
                CUT(200 + 10 * t + 7)
                TT(dve, xdt_tm[:].rearrange("p (h c) -> p h c", h=16), xs_tm[:].rearrange("p (h c) -> p h c", h=16),
                   bc(gs[:, t, 0:16].unsqueeze(2), [128, 16, 64]), ALU.mult, [t_xs, t_gs], [t_xdt])
                for g in range(2):
                    TR(bankbf(0)[:, g * 128:(g + 1) * 128], cvS[:, 8 + g, tc_], identb[:], [t_cvS, t_cb], [TB[0]])
                CP(act, B_tm[:], bankbf(0)[:, 0:256], [TB[0]], [t_Btm])
                CUT(200 + 10 * t + 1)
                smp = bank(3)
                MM(smp[:, 0:16], trif, gs[:, t, 16:32], True, True, [t_c, t_gs], [TB[3]])
                MM(smp[:, 16:32], onesf, gs[:, t, 16:32], True, True, [t_c, t_gs], [TB[3]])
                for g in range(2):
                    MM(smp[0:8, 32 + g * 128: 160 + g * 128], gs[:, t, 16 + g * 8:24 + g * 8], trif, True, True,
                       [t_c, t_gs], [TB[3]])
                CP(dve, sm[:, 0:32], smp[:, 0:32], [TB[3]], [t_sm])
                TS(dve, csT[:, 0:2, :].rearrange("p a b -> p (a b)"), smp[0:8, 32:288], -1.0, ALU.mult, [TB[3]], [t_csT])
                ACT(sm[:, 32:48], sm[:, 0:16], AF.Exp, [t_sm], [t_sm])
                TT(dve, sm[:, 48:64], sm[:, 16:32], sm[:, 0:16], ALU.subtract, [t_sm], [t_sm])
                ACT(sm[:, 48:64], sm[:, 48:64], AF.Exp, [t_sm], [t_sm])
                ACT(sm[:, 64:80], sm[:, 16:32], AF.Exp, [t_sm], [t_sm])
                CUT(200 + 10 * t + 2)
                for g in range(2):
                    MM(bank(6 + g), cvS[:, 10 + g, tc_], stSb[:, g * 512:(g + 1) * 512], True, True,
                       [t_cvS, t_stSb], [TB[6 + g]])
                CUT(200 + 10 * t + 3)
                for g in range(2):
                    TT(dve, bd[:], sel8.rearrange("p (h l) -> p h l", h=8),
                       bc(csT[:, g, :].unsqueeze(1), [8, 8, 128]), ALU.mult, [t_c, t_csT], [t_bd])
                    for hb in range(2):
                        eb_ = bank(4 + hb)
                        cs_ = slice(hb * 512, (hb + 1) * 512)
                        MM(eb_, csT[:, g, :], sel8[:, cs_], True, False, [t_csT, t_c], [TB[4 + hb]])
                        MM(eb_, csT[:, 3, :], bd[:].rearrange("p h l -> p (h l)")[:, cs_], False, False,
                           [t_bd, t_csT], [TB[4 + hb]])
                        MM(eb_, identb[:], maskb[:, hb * 4:(hb + 1) * 4, :].rearrange("p h l -> p (h l)"), False, True,
                           [t_cb], [TB[4 + hb]])
                    ACT(Lt[:].rearrange("p h l -> p (h l)"), PS[:, 4 * 512:6 * 512], AF.Exp, [TB[4], TB[5]], [t_Lt])
                    if g == 1:
                        CUT(200 + 10 * t + 4)
                    MM(bank(3, 128), cvS[:, 8 + g, tc_], cvS[:, 10 + g, tc_], True, True, [t_cvS], [TB[3]])
                    CP(act, cbt[:], bank(3, 128), [TB[3]], [t_cbt])
                    TT(dve, Mt[:], Lt[:], bc(cbt[:].unsqueeze(1), [128, 8, 128]), ALU.mult, [t_Lt, t_cbt], [t_Mt])
                    for hh in range(8):
                        h = g * 8 + hh
                        ob = bank(1 + g)[:, hh * 64:(hh + 1) * 64]
                        MM(ob, Mt[:, hh, :], xdt_tm[:, h * 64:(h + 1) * 64], True, False, [t_Mt, t_xdt], [TB[1 + g]])
                        MM(ob, dgD[:, h, :], xs_tm[:, h * 64:(h + 1) * 64], False, True, [t_dg, t_xs], [TB[1 + g]])
                for hh in range(8):
                    TR(bankbf(0)[:, hh * 128:(hh + 1) * 128], cvM[:, 8 + hh, tc_], identb[:], [t_cvM, t_cb], [TB[0]])
                CP(act, k_tm[:], bankbf(0), [TB[0]], [t_ktm])
                smp = bank(3)
                MM(smp[:, 0:8], trif, gs[:, t, 32:40], True, True, [t_c, t_gs], [TB[3]])
                MM(smp[:, 8:16], onesf, gs[:, t, 32:40], True, True, [t_c, t_gs], [TB[3]])
                MM(smp[0:8, 32:160], gs[:, t, 40:48], identf, True, False, [t_c, t_gs], [TB[3]])
                MM(smp[0:8, 32:160], gs[:, t, 32:40], trif, False, True, [t_c, t_gs], [TB[3]])
                MM(smp[0:8, 160:288], gs[:, t, 32:40], trif, True, True, [t_c, t_gs], [TB[3]])
                CP(dve, sm[:, 80:96], smp[:, 0:16], [TB[3]], [t_sm])
                CP(dve, csT[:, 2, :], smp[0:8, 32:160], [TB[3]], [t_csT])
                ACT(sm[:, 96:104], sm[:, 80:88], AF.Exp, [t_sm], [t_sm], scale=-1.0)
                TT(dve, sm[:, 104:112], sm[:, 80:88], sm[:, 88:96], ALU.subtract, [t_sm], [t_sm])
                TT(dve, sm[:, 104:112], sm[:, 104:112], gs[:, t, 40:48], ALU.add, [t_sm, t_gs], [t_sm])
                ACT(sm[:, 104:112], sm[:, 104:112], AF.Exp, [t_sm], [t_sm])
                ACT(sm[:, 112:120], sm[:, 88:96], AF.Exp, [t_sm], [t_sm], scale=-1.0)
                TT(dve, bd[:], sel8.rearrange("p (h l) -> p h l", h=8),
                   bc(smp[0:8, 160:288].unsqueeze(1), [8, 8, 128]), ALU.mult, [t_c, TB[3]], [t_bd])
                for hb in range(2):
                    eb_ = bank(4 + hb)
                    cs_ = slice(hb * 512, (hb + 1) * 512)
                    MM(eb_, csT[:, 2, :], sel8[:, cs_], True, False, [t_csT, t_c], [TB[4 + hb]])
                    MM(eb_, csT[:, 3, :], bd[:].rearrange("p h l -> p (h l)")[:, cs_], False, False,
                       [t_bd, t_csT], [TB[4 + hb]])
                    MM(eb_, identb[:], maskb[:, hb * 4:(hb + 1) * 4, :].rearrange("p h l -> p (h l)"), False, True,
                       [t_cb], [TB[4 + hb]])
                ACT(Lt[:].rearrange("p h l -> p (h l)"), PS[:, 4 * 512:6 * 512], AF.Exp, [TB[4], TB[5]], [t_Lt])
                CUT(200 + 10 * t + 5)
                TT(dve, fa[:].rearrange("p (h c) -> p h c", h=16), PS[:, 6 * 512:8 * 512].rearrange("p (h c) -> p h c", h=16),
                   bc(sm[:, 32:48].unsqueeze(2), [128, 16, 64]), ALU.mult, [TB[6], TB[7], t_sm], [t_fa])
                TT(dve, fa[:], PS[:, 1 * 512:3 * 512], fa[:], ALU.add, [TB[1], TB[2], t_fa], [t_fa])
                TT(dve, fa[:], fa[:], zs[:, t, :], ALU.mult, [t_fa, t_zs], [t_fa])
                for g in range(2):
                    ACT(junk[:, g * 512:(g + 1) * 512], fa[:, g * 512:(g + 1) * 512], AF.Square, [t_fa], [t_junk, t_st4],
                        accum_out=st4[:, 4 + g:5 + g])
                ACT(st4[:, 6:8], st4[:, 4:6], AF.Ln, [t_st4], [t_st4], scale=1.0 / 512, bias=EPS)
                ACT(st4[:, 8:10], st4[:, 6:8], AF.Exp, [t_st4], [t_st4], scale=-0.5)
                for g in range(2):
                    STT(ynb[:, g * 512:(g + 1) * 512], fa[:, g * 512:(g + 1) * 512], st4[:, 8 + g:9 + g],
                        snw[:, g * 512:(g + 1) * 512], ALU.mult, ALU.mult, [t_fa, t_st4, t_nw], [t_ynb])
                for cc in range(8):
                    TR(bankbf(0)[:, cc * 128:(cc + 1) * 128], ynb[:, cc * 128:(cc + 1) * 128], identb[:], [t_ynb, t_cb], [TB[0]])
                CP(act, mixT[:, 0:8, :].rearrange("p a b -> p (a b)"), bankbf(0), [TB[0]], [t_mixT])
                CUT(200 + 10 * t + 6)
                TT(dve, xdec[:].rearrange("p (h c) -> p h c", h=16), xdt_tm[:].rearrange("p (h c) -> p h c", h=16),
                   bc(sm[:, 48:64].unsqueeze(2), [128, 16, 64]), ALU.mult, [t_xdt, t_sm], [t_xdec])
                for g in range(2):
                    MM(bank(6 + g), B_tm[:, g * 128:(g + 1) * 128], xdec[:, g * 512:(g + 1) * 512], True, True,
                       [t_Btm, t_xdec], [TB[6 + g]])
                TT(dve, stS[:].rearrange("p (h c) -> p h c", h=16), stS[:].rearrange("p (h c) -> p h c", h=16),
                   bc(sm[:, 64:80].unsqueeze(2), [128, 16, 64]), ALU.mult, [t_stS, t_sm], [t_stS])
                TT(dve, stS[:], stS[:], PS[:, 6 * 512:8 * 512], ALU.add, [t_stS, TB[6], TB[7]], [t_stS])
                CP(act, stSb[:], stS[:], [t_stS], [t_stSb])

                CUT(4)
                CUT(100 + 10 * t + 4)
                if 'ml' not in _os.environ.get('KSKIP', ''):
                    for hh in range(8):
                        b = 6 + hh // 4
                        MM(bank(b)[:, (hh % 4) * 128:(hh % 4 + 1) * 128], cvM[:, 8 + hh, tc_], cvM[:, hh, tc_], True, True,
                           [t_cvM], [TB[b]])
                    TT(dve, Mt[:].rearrange("p h l -> p (h l)"), Lt[:].rearrange("p h l -> p (h l)"), PS[:, 6 * 512:8 * 512],
                       ALU.mult, [t_Lt, TB[6], TB[7]], [t_Mt])
                    for hh in range(8):
                        b = 1 + hh // 4
                        MM(bank(b)[:, (hh % 4) * 128:(hh % 4 + 1) * 128], Mt[:, hh, :], vtm[:, t, hh * 128:(hh + 1) * 128],
                           True, True, [t_Mt, t_v], [TB[b]])
                    for hh in range(8):
                        b = 6 + hh // 4
                        MM(bank(b)[:, (hh % 4) * 128:(hh % 4 + 1) * 128], cvM[:, hh, tc_], Cmb[:, hh * 128:(hh + 1) * 128],
                           True, True, [t_cvM, t_Cmb], [TB[b]])
                    for hh in range(8):
                        MM(smp[:, 300 + hh:301 + hh], Mt[:, hh, :], onesb[:, 0:1], True, True, [t_Mt, t_cb], [TB[3]])
                    for hh in range(8):
                        MM(smp[:, 308 + hh:309 + hh], cvM[:, hh, tc_], nmb[:, hh:hh + 1], True, True, [t_cvM, t_nmb], [TB[3]])
                    TT(dve, sm[:, 120:128], smp[:, 308:316], sm[:, 96:104], ALU.mult, [TB[3], t_sm], [t_sm])
                    TT(dve, sm[:, 120:128], sm[:, 120:128], smp[:, 300:308], ALU.add, [TB[3], t_sm], [t_sm])
                    TT(dve, fb[:].rearrange("p (h c) -> p h c", h=8), PS[:, 6 * 512:8 * 512].rearrange("p (h c) -> p h c", h=8),
                       bc(sm[:, 96:104].unsqueeze(2), [128, 8, 128]), ALU.mult, [TB[6], TB[7], t_sm], [t_fb])
                    TT(dve, fb[:], PS[:, 1 * 512:3 * 512], fb[:], ALU.add, [TB[1], TB[2], t_fb], [t_fb])
                    RED(sm[:, 128:136], fb[:].rearrange("p (h c) -> p h c", h=8), ALU.add, [t_fb], [t_sm])
                    ACT(fa[:], fb[:], AF.Square, [t_fb], [t_fa])
                    RED(sm[:, 136:144], fa[:].rearrange("p (h c) -> p h c", h=8), ALU.add, [t_fa], [t_sm])
                    TS(dve, sm[:, 128:136], sm[:, 128:136], 1.0 / 128, ALU.mult, [t_sm], [t_sm])
                    TT(dve, sm[:, 144:152], sm[:, 128:136], sm[:, 128:136], ALU.mult, [t_sm], [t_sm])
                    STT(sm[:, 136:144], sm[:, 136:144], 1.0 / 128, sm[:, 144:152], ALU.mult, ALU.subtract, [t_sm], [t_sm])
                    TT(dve, sm[:, 144:152], sm[:, 120:128], sm[:, 120:128], ALU.mult, [t_sm], [t_sm])
                    TS(dve, sm[:, 144:152], sm[:, 144:152], 1.0, ALU.max, [t_sm], [t_sm])
                    STT(sm[:, 136:144], sm[:, 144:152], EPS, sm[:, 136:144], ALU.mult, ALU.add, [t_sm], [t_sm])
                    ACT(sm[:, 136:144], sm[:, 136:144], AF.Ln, [t_sm], [t_sm])
                    ACT(sm[:, 136:144], sm[:, 136:144], AF.Exp, [t_sm], [t_sm], scale=-0.5)
                    TT(dve, fb[:].rearrange("p (h c) -> p h c", h=8), fb[:].rearrange("p (h c) -> p h c", h=8),
                       bc(sm[:, 128:136].unsqueeze(2), [128, 8, 128]), ALU.subtract, [t_fb, t_sm], [t_fb])
                    TT(dve, fb[:].rearrange("p (h c) -> p h c", h=8), fb[:].rearrange("p (h c) -> p h c", h=8),
                       bc(sm[:, 136:144].unsqueeze(2), [128, 8, 128]), ALU.mult, [t_fb, t_sm], [t_fb])
                    TT(pool, fa[:], so[:, t, :], mnw[:], ALU.mult, [t_so, t_nw], [t_fa])
                    TT(dve, ynb[:], fb[:], fa[:], ALU.mult, [t_fb, t_fa], [t_ynb])
                    for cc in range(8):
                        TR(bankbf(0)[:, cc * 128:(cc + 1) * 128], ynb[:, cc * 128:(cc + 1) * 128], identb[:], [t_ynb, t_cb], [TB[0]])
                    CP(act, mixT[:, 8:16, :].rearrange("p a b -> p (a b)"), bankbf(0), [TB[0]], [t_mixT])
                    TT(dve, kk[:].rearrange("p (h c) -> p h c", h=8), k_tm[:].rearrange("p (h c) -> p h c", h=8),
                       bc(sm[:, 104:112].unsqueeze(2), [128, 8, 128]), ALU.mult, [t_ktm, t_sm], [t_kk])
                    for hh in range(8):
                        b = 6 + hh // 4
                        MM(bank(b)[:, (hh % 4) * 128:(hh % 4 + 1) * 128], kk[:, hh * 128:(hh + 1) * 128],
                           vtm[:, t, hh * 128:(hh + 1) * 128], True, True, [t_kk, t_v], [TB[b]])
                    for hh in range(8):
                        MM(smp[:, 320 + hh:321 + hh], kk[:, hh * 128:(hh + 1) * 128], onesb[:, 0:1], True, True,
                           [t_kk, t_cb], [TB[3]])
                    TT(dve, Cm[:].rearrange("p (h c) -> p h c", h=8), Cm[:].rearrange("p (h c) -> p h c", h=8),
                       bc(sm[:, 112:120].unsqueeze(2), [128, 8, 128]), ALU.mult, [t_Cm, t_sm], [t_Cm])
                    TT(dve, Cm[:], Cm[:], PS[:, 6 * 512:8 * 512], ALU.add, [t_Cm, TB[6], TB[7]], [t_Cm])
                    CP(act, Cmb[:], Cm[:], [t_Cm], [t_Cmb])
                    TT(dve, nm[:], nm[:], sm[:, 112:120], ALU.mult, [t_nm, t_sm], [t_nm])
                    TT(dve, nm[:], nm[:], smp[:, 320:328], ALU.add, [t_nm, TB[3]], [t_nm])
                    CP(dve, nmb[:], nm[:], [t_nm], [t_nmb])

                CUT(5)
                CUT(100 + 10 * t + 5)
                if 'out' not in _os.environ.get('KSKIP', ''):
                    tk = tok0 + t * 128
                    for nh in range(2):
                        for e in range(16):
                            MM(bank(4 + nh), mixT[:, e, :], wout[:, e, nh * 512:(nh + 1) * 512], e == 0, e == 15,
                               [t_mixT, t_wout], [TB[4 + nh]])
                    sp.dma(xt[:], x_d[tk:tk + 128, :], W=[t_xt])
                    TT(dve, fa[:], PS[:, 4 * 512:6 * 512], xt[:], ALU.add, [TB[4], TB[5], t_xt], [t_fa])
                    sp.dma(x1_d[tk:tk + 128, :], fa[:], R=[t_fa])
                    ACT(junk[:], fa[:], AF.Square, [t_fa], [t_junk, t_st4], accum_out=st4[:, 10:11])
                    ACT(st4[:, 11:12], st4[:, 10:11], AF.Ln, [t_st4], [t_st4], scale=1.0 / D, bias=EPS)
                    ACT(st4[:, 12:13], st4[:, 11:12], AF.Exp, [t_st4], [t_st4], scale=-0.5)
                    TS(dve, fb[:], fa[:], st4[:, 12:13], ALU.mult, [t_fa, t_st4], [t_fb])
                    CP(pool, xhb[:], fb[:], [t_fb], [t_xhb])
                    sp.dma(h2_d[tk:tk + 128, :], xhb[:], R=[t_xhb])
                    for kc in range(8):
                        b = 1 + kc // 4
                        TR(bank(b)[:, (kc % 4) * 128:(kc % 4 + 1) * 128], fb[:, kc * 128:(kc + 1) * 128], identf, [t_fb, t_c], [TB[b]])
                    TT(dve, h2T[:], PS[:, 1 * 512:3 * 512].rearrange("p (k c) -> p k c", k=8),
                       bc(n2w[:].unsqueeze(2), [128, 8, 128]), ALU.mult, [TB[1], TB[2], t_par], [t_h2T])
                    for kc in range(8):
                        MM(bank(3, 36), h2T[:, kc, :], wr[:, kc, :], kc == 0, kc == 7, [t_h2T, t_par], [TB[3]])
                    TT(dve, lgt[:], bank(3, 36), rb_b, ALU.add, [TB[3], t_rep], [t_lgt])
                    sp.dma(lg_d[tk:tk + 128, :], lgt[:], R=[t_lgt])
                CUT(6)
                CUT(100 + 10 * t + 6)
                if _os.environ.get("KBAR", "0") == "1":
                    K.barrier()

    K.barrier()
    if stop_after == "p1":
        return K
    K.release(ph1)

    IOA = bass.IndirectOffsetOnAxis
    NTL = NTILE
    Wd = NTL * 32
    lg = K.sb([128, NTL, 36], F32, "lg")
    t_lg = Tok()
    sp.dma(lg[:], lg_d.rearrange("(j p) c -> p j c", p=128), W=[t_lg])
    nfw = K.sb([128, D], F32, "nfw")
    t_nfw = Tok()
    sp.dma(nfw[:], nfw_d, W=[t_nfw])
    zt = K.sb([128, 2048], BF16, "zt")
    t_zt = Tok()
    pool.do(lambda: nc.gpsimd.memset(zt[:], 0.0), W=[t_zt])
    zview = h2s_d.rearrange("(n p r) d -> n p (r d)", p=128, r=2)
    t_zero = []
    for n in range(NROW // 256):
        tz = Tok()
        sp.dma(zview[n], zt[:], R=[t_zt], W=[tz])
        t_zero.append(tz)

    def rt(shape, dt=F32, name=None):
        return K.sb(shape, dt, name)
    t_r = Tok()
    gts = rt([128, 2, NTL]); desti = rt([128, 2, NTL], I32); idxG = rt([128, NSLOT, 8], I32); idxD = rt([128, NSLOT, 4], I32)
    hj = [K.sb([128, D], BF16, f"hj{i}") for i in range(2)]
    rmark = K.mark()
    gmx = rt([128, NTL]); ohg = rt([128, NTL, 4]); eg = rt([128, NTL, 4]); sg = rt([128, NTL]); pg = rt([128, NTL])
    pen = rt([128, NTL, 4]); me = rt([128, NTL, 32]); me2 = rt([128, NTL, 32]); oh1 = rt([128, NTL, 32]); oh2 = rt([128, NTL, 32])
    v1 = rt([128, NTL]); v2 = rt([128, NTL]); tmpw = rt([128, NTL, 32])
    OHb = rt([128, Wd], BF16); PCs = rt([128, NTL, 32]); Ts = rt([128, NTL, 32]); Rr = rt([128, NTL, 32])
    cnt = rt([128, 32]); cmp_ = rt([128, 32, 64]); nsl = rt([128, 32]); bsl = rt([128, 32]); incl = rt([128, 32]); base = rt([128, 32])
    destf = rt([128, 2, NTL]); cmp2 = rt([128, NSLOT, 32]); eslot = rt([128, NSLOT])
    idxf = rt([128, NSLOT, 8])
    gl = lg[:, :, 0:4]
    el = lg[:, :, 4:36]
    RED(gmx[:], gl, ALU.max, [t_lg], [t_r])
    TT(dve, ohg[:], gl, bc(gmx[:].unsqueeze(2), [128, NTL, 4]), ALU.is_equal, [t_lg, t_r], [t_r])
    TT(dve, eg[:], gl, bc(gmx[:].unsqueeze(2), [128, NTL, 4]), ALU.subtract, [t_lg, t_r], [t_r])
    ACT(eg[:], eg[:], AF.Exp, [t_r], [t_r])
    RED(sg[:], eg[:], ALU.add, [t_r], [t_r])
    dve.do(lambda: nc.vector.reciprocal(out=pg[:], in_=sg[:]), [t_r], [t_r])
    TS(dve, pen[:], ohg[:], -1.0, ALU.add, [t_r], [t_r], s2=1e30, op1=ALU.mult)
    TT(dve, me[:].rearrange("p j (g e) -> p j g e", g=4), el.rearrange("p j (g e) -> p j g e", g=4),
       bc(pen[:].unsqueeze(3), [128, NTL, 4, 8]), ALU.add, [t_lg, t_r], [t_r])
    RED(v1[:], me[:], ALU.max, [t_r], [t_r])
    TT(dve, oh1[:], me[:], bc(v1[:].unsqueeze(2), [128, NTL, 32]), ALU.is_equal, [t_r], [t_r])
    STT(me2[:].rearrange("p j e -> p (j e)"), oh1[:].rearrange("p j e -> p (j e)"), -1e30,
        me[:].rearrange("p j e -> p (j e)"), ALU.mult, ALU.add, [t_r], [t_r])
    RED(v2[:], me2[:], ALU.max, [t_r], [t_r])
    TT(dve, oh2[:], me2[:], bc(v2[:].unsqueeze(2), [128, NTL, 32]), ALU.is_equal, [t_r], [t_r])
    TT(dve, v2[:], v2[:], v1[:], ALU.subtract, [t_r], [t_r])
    ACT(v2[:], v2[:], AF.Exp, [t_r], [t_r])
    TS(dve, v2[:], v2[:], 1.0, ALU.add, [t_r], [t_r])
    dve.do(lambda: nc.vector.reciprocal(out=v2[:], in_=v2[:]), [t_r], [t_r])
    TT(dve, gts[:, 0, :], pg[:], v2[:], ALU.mult, [t_r], [t_r])
    TT(dve, gts[:, 1, :], pg[:], gts[:, 0, :], ALU.subtract, [t_r], [t_r])
    TT(dve, OHb[:], oh1[:].rearrange("p j e -> p (j e)"), oh2[:].rearrange("p j e -> p (j e)"), ALU.add, [t_r], [t_r])
    nchk = (Wd + 511) // 512
    for c in range(nchk):
        w_ = min(512, Wd - c * 512)
        MM(bank(c, w_), ustrb[:], OHb[:, c * 512:c * 512 + w_], True, True, [t_r, t_cb], [TB[c]])
        MM(bank(4 + c, w_), onesb[:], OHb[:, c * 512:c * 512 + w_], True, True, [t_r, t_cb], [TB[4 + c]])
    CP(dve, PCs[:].rearrange("p j e -> p (j e)"), PS[:, 0:Wd], [TB[c_] for c_ in range(nchk)], [t_r])
    CP(dve, Ts[:].rearrange("p j e -> p (j e)"), PS[:, 2048:2048 + Wd], [TB[4 + c_] for c_ in range(nchk)], [t_r])
    dve.do(lambda: nc.vector.memset(Rr[:, 0, :], 0.0), W=[t_r])
    for j in range(1, NTL):
        TT(dve, Rr[:, j, :], Rr[:, j - 1, :], Ts[:, j - 1, :], ALU.add, [t_r], [t_r])
    TT(dve, cnt[:], Rr[:, NTL - 1, :], Ts[:, NTL - 1, :], ALU.add, [t_r], [t_r])
    TT(dve, PCs[:], PCs[:], Rr[:], ALU.add, [t_r], [t_r])
    TT(dve, tmpw[:], oh1[:], PCs[:], ALU.mult, [t_r], [t_r])
    RED(destf[:, 0, :], tmpw[:], ALU.add, [t_r], [t_r])
    TT(dve, tmpw[:], oh2[:], PCs[:], ALU.mult, [t_r], [t_r])
    RED(destf[:, 1, :], tmpw[:], ALU.add, [t_r], [t_r])
    TT(dve, cmp_[:], bc(thr.unsqueeze(1), [128, 32, 64]), bc(cnt[:].unsqueeze(2), [128, 32, 64]), ALU.is_lt, [t_r, t_c], [t_r])
    RED(nsl[:], cmp_[:], ALU.add, [t_r], [t_r])
    dve.do(lambda: nc.vector.memset(bsl[:, 0:1], 0.0), W=[t_r])
    for e in range(1, 32):
        TT(dve, bsl[:, e:e + 1], bsl[:, e - 1:e], nsl[:, e - 1:e], ALU.add, [t_r], [t_r])
    TT(dve, incl[:], bsl[:], nsl[:], ALU.add, [t_r], [t_r])
    TS(dve, base[:], bsl[:], float(SL), ALU.mult, [t_r], [t_r])
    for k, ohk in ((0, oh1), (1, oh2)):
        TT(dve, tmpw[:], ohk[:], bc(base[:].unsqueeze(1), [128, NTL, 32]), ALU.mult, [t_r], [t_r])
        RED(v1[:], tmpw[:], ALU.add, [t_r], [t_r])
        TT(dve, destf[:, k, :], destf[:, k, :], v1[:], ALU.add, [t_r], [t_r])
    CP(dve, desti[:], destf[:], [t_r], [t_r])
    TT(dve, cmp2[:], bc(incl[:].unsqueeze(1), [128, NSLOT, 32]), bc(slotid[:, 0:NSLOT].unsqueeze(2), [128, NSLOT, 32]),
       ALU.is_le, [t_r, t_c], [t_r])
    RED(eslot[:], cmp2[:], ALU.add, [t_r], [t_r])
    TS(dve, eslot[:], eslot[:], 31.0, ALU.min, [t_r], [t_r])
    TS(dve, eslot[:], eslot[:], 128.0, ALU.mult, [t_r], [t_r], s2=rowoff[:, 0:1], op1=ALU.add)
    CP(dve, idxG[:, :, 0], eslot[:], [t_r], [t_r])
    K.barrier()
    K.release(rmark)

    t_scat = []
    t_hj = [Tok() for _ in range(2)]
    for j in range(NTL):
        sp.dma(hj[j % 2][:], h2_d[j * 128:(j + 1) * 128, :], W=[t_hj[j % 2]])
        for k in range(2):
            ts_ = Tok()
            pool.dma(h2s_d[:, :], hj[j % 2][:], R=[t_r, t_hj[j % 2]] + t_zero, W=[ts_],
                     indirect=dict(out_offset=IOA(ap=desti[:, k, j:j + 1], axis=0), in_offset=None))
            t_scat.append(ts_)

    wgb = [K.sb([128, 8, 512], BF16, f"wgb{i}") for i in range(2)]
    wub = [K.sb([128, 8, 512], BF16, f"wub{i}") for i in range(2)]
    wdb = [K.sb([128, 4, D], BF16, f"wdb{i}") for i in range(2)]
    t_w = [Tok() for _ in range(2)]
    hsb = [K.sb([128, 4, D], BF16, f"hs{i}") for i in range(2)]
    t_hsb = [Tok() for _ in range(2)]

    def load_hs(s):
        sp.dma(hsb[s % 2][:], h2s_d[s * SL:(s + 1) * SL, :].rearrange("(r p) d -> p r d", p=128), R=t_scat, W=[t_hsb[s % 2]])
    hTb = [K.sb([128, 8, 512], BF16, f"hT{i}") for i in range(2)]
    t_hTb = [Tok() for _ in range(2)]
    sgb = [K.sb([128, 512], BF16, f"sgb{i}") for i in range(2)]
    t_sgb = [Tok() for _ in range(2)]
    aT = K.sb([128, 4, 512], BF16, "aT")
    t_aT = [Tok() for _ in range(4)]
    yst = [K.sb([128, D], F32, f"yst{i}") for i in range(2)]
    t_yst = [Tok() for _ in range(2)]
    t_ys = []

    def load_w(s):
        q = s % 2
        pool.dma(wgb[q][:].rearrange("p k f -> p (k f)"), wg_d, R=[t_r], W=[t_w[q]],
                 indirect=dict(out_offset=None, in_offset=IOA(ap=idxG[:, s, 0:1], axis=0)))
        pool.dma(wub[q][:].rearrange("p k f -> p (k f)"), wu_d, R=[t_r], W=[t_w[q]],
                 indirect=dict(out_offset=None, in_offset=IOA(ap=idxG[:, s, 0:1], axis=0)))
        pool.dma(wdb[q][:].rearrange("p k f -> p (k f)"), wd_d, R=[t_r], W=[t_w[q]],
                 indirect=dict(out_offset=None, in_offset=IOA(ap=idxG[:, s, 0:1], axis=0)))

    def do_tr(s_):
        q_ = s_ % 2
        for r in range(4):
            b0 = 0 if r % 2 == 0 else 7
            for kc in range(8):
                TR(bankbf(b0)[:, kc * 128:(kc + 1) * 128], hsb[q_][:, r, kc * 128:(kc + 1) * 128], identb[:],
                   [t_hsb[q_], t_cb], [TB[b0]])
            TT(dve, hTb[q_][:, :, r * 128:(r + 1) * 128], bankbf(b0).rearrange("p (k c) -> p k c", k=8),
               bc(n2w[:].unsqueeze(2), [128, 8, 128]), ALU.mult, [TB[b0], t_par], [t_hTb[q_]])

    load_w(0)
    load_hs(0)
    yq = 0
    for s in range(NSLOT):
        q = s % 2
        hs = hsb[q]
        t_hs = t_hsb[q]
        if s + 1 < NSLOT:
            load_w(s + 1)
            load_hs(s + 1)
        if s == 0:
            do_tr(0)
        hT = hTb[q]
        t_hT = t_hTb[q]
        for fc in range(4):
            bg = [1, 3][fc % 2]
            bu = [2, 6][fc % 2]
            for kc in range(8):
                MM(bank(bg), wgb[q][:, kc, fc * 128:(fc + 1) * 128], hT[:, kc, :], kc == 0, kc == 7, [t_w[q], t_hT], [TB[bg]])
            for kc in range(8):
                MM(bank(bu), wub[q][:, kc, fc * 128:(fc + 1) * 128], hT[:, kc, :], kc == 0, kc == 7, [t_w[q], t_hT], [TB[bu]])
            ACT(sgb[fc % 2][:], bank(bg), AF.Silu, [TB[bg]], [t_sgb[fc % 2]])
            TT(dve, aT[:, fc, :], sgb[fc % 2][:], bank(bu), ALU.mult, [t_sgb[fc % 2], TB[bu]], [t_aT[fc]])
        if s + 1 < NSLOT:
            do_tr(s + 1)
        for r in range(4):
            db = 4 if r % 2 == 0 else 6
            for nh in range(2):
                for fc in range(4):
                    MM(bank(db + nh), aT[:, fc, r * 128:(r + 1) * 128], wdb[q][:, fc, nh * 512:(nh + 1) * 512], fc == 0, fc == 3,
                       [t_aT[fc], t_w[q]], [TB[db + nh]])
            CP(act, yst[yq][:], PS[:, db * 512:(db + 2) * 512], [TB[db], TB[db + 1]], [t_yst[yq]])
            ty = Tok()
            sp.dma(ys_d[s * SL + r * 128: s * SL + (r + 1) * 128, :], yst[yq][:], R=[t_yst[yq]], W=[ty])
            t_ys.append(ty)
            yq ^= 1

    NB2 = 2
    ya = [K.sb([128, D], F32, f"ya{i}") for i in range(NB2)]
    yb = [K.sb([128, D], F32, f"yb{i}") for i in range(NB2)]
    x1t = [K.sb([128, D], F32, f"x1t{i}") for i in range(NB2)]
    acc = [K.sb([128, D], F32, f"acc{i}") for i in range(NB2)]
    ot = [K.sb([128, D], F32, f"ot{i}") for i in range(NB2)]
    jk = K.sb([128, D], BF16, "jk")
    s5 = K.sb([128, 4 * NB2], F32, "s5")
    t_ya = [Tok() for _ in range(NB2)]
    t_yb = [Tok() for _ in range(NB2)]
    t_x1t = [Tok() for _ in range(NB2)]
    t_acc = [Tok() for _ in range(NB2)]
    t_ot = [Tok() for _ in range(NB2)]
    t_s5 = [Tok() for _ in range(NB2)]
    t_jk = Tok()
    t_out = []
    for j in range(NTL):
        u = j % NB2
        c0 = 4 * u
        pool.dma(ya[u][:], ys_d, R=[t_r] + t_ys, W=[t_ya[u]],
                 indirect=dict(out_offset=None, in_offset=IOA(ap=desti[:, 0, j:j + 1], axis=0)))
        pool.dma(yb[u][:], ys_d, R=[t_r] + t_ys, W=[t_yb[u]],
                 indirect=dict(out_offset=None, in_offset=IOA(ap=desti[:, 1, j:j + 1], axis=0)))
        sp.dma(x1t[u][:], x1_d[j * 128:(j + 1) * 128, :], W=[t_x1t[u]])
        STT(acc[u][:], ya[u][:], gts[:, 0, j:j + 1], x1t[u][:], ALU.mult, ALU.add, [t_ya[u], t_x1t[u], t_r], [t_acc[u]])
        STT(acc[u][:], yb[u][:], gts[:, 1, j:j + 1], acc[u][:], ALU.mult, ALU.add, [t_yb[u], t_acc[u], t_r], [t_acc[u]])
        ACT(jk[:], acc[u][:], AF.Square, [t_acc[u]], [t_jk, t_s5[u]], accum_out=s5[:, c0:c0 + 1])
        ACT(s5[:, c0 + 1:c0 + 2], s5[:, c0:c0 + 1], AF.Ln, [t_s5[u]], [t_s5[u]], scale=1.0 / D, bias=EPS)
        ACT(s5[:, c0 + 2:c0 + 3], s5[:, c0 + 1:c0 + 2], AF.Exp, [t_s5[u]], [t_s5[u]], scale=-0.5)
        STT(ot[u][:], acc[u][:], s5[:, c0 + 2:c0 + 3], nfw[:], ALU.mult, ALU.mult, [t_acc[u], t_s5[u], t_nfw], [t_ot[u]])
        to = Tok()
        sp.dma(out_d[j * 128:(j + 1) * 128, :], ot[u][:], R=[t_ot[u]], W=[to])
        t_out.append(to)
    K.barrier()
    return K


def _shared_inputs(inp):
    f = lambda a: np.ascontiguousarray(np.asarray(a, dtype=np.float32))
    w_in = f(inp["w_in"])[0]
    order = np.concatenate([np.arange(1024, 2560), np.arange(0, 1024), np.arange(2560, 2576),
                            np.arange(6672, 6688), np.arange(2576, 4624), np.arange(4624, 5648),
                            np.arange(5648, 6672)])
    w_in_p = w_in[:, order].reshape(8, 128, NCOLS).transpose(1, 0, 2)
    w_out = f(inp["w_out"])[0].reshape(16, 128, D).transpose(1, 0, 2)
    cwf = np.concatenate([f(inp["ssd_conv_w"])[0], f(inp["ml_conv_w"])[0]], axis=1)
    cw = cwf.T.reshape(28, 128, 4).transpose(1, 0, 2)
    cbf = np.concatenate([f(inp["ssd_conv_b"])[0], f(inp["ml_conv_b"])[0]])
    cb = cbf.reshape(28, 128).T
    n1w = f(inp["norm1_w"])[0].reshape(8, 128).T
    n2w = f(inp["norm2_w"])[0].reshape(8, 128).T
    repv = np.concatenate([f(inp["ssd_dt_bias"])[0], f(inp["ssd_a_log"])[0], f(inp["ssd_d"])[0],
                           f(inp["ml_i_bias"])[0], f(inp["ml_f_bias"])[0],
                           f(inp["router_g_b"])[0], f(inp["router_e_b"])[0]])
    rep = np.tile(repv[None, :], (128, 1))
    snw = np.tile(f(inp["ssd_norm_w"])[0][None, :], (128, 1))
    mnw = np.tile(f(inp["ml_norm_w"])[0][None, :], (128, 1))
    nfw = np.tile(f(inp["norm_f_w"])[None, :], (128, 1))
    wr = np.concatenate([f(inp["router_g_w"])[0], f(inp["router_e_w"])[0]], axis=1).reshape(8, 128, 36).transpose(1, 0, 2)
    s = np.arange(128)
    ident = np.eye(128, dtype=np.float32)
    tri = (s[:, None] <= s[None, :]).astype(np.float32)
    ones = np.ones((128, 128), np.float32)
    ustr = (s[:, None] < s[None, :]).astype(np.float32)
    sel = np.zeros((128, 8, 128), np.float32)
    for h in range(8):
        sel[h, h, :] = 1.0
    thr = np.tile((np.arange(64, dtype=np.float32) * SL)[None, :], (128, 1))
    slotid = np.tile(np.arange(128, dtype=np.float32)[None, :], (128, 1))
    rowoff = (np.arange(24, dtype=np.float32)[None, :] * 128 + s[:, None]).astype(np.float32)
    cst = np.concatenate([ident, tri, ones, ustr, sel.reshape(128, 1024), thr, slotid, rowoff], axis=1)
    d = dict(w_in=w_in_p, w_out=w_out, cw=cw, cb=cb, n1w=n1w, n2w=n2w, rep=rep, snw=snw, mnw=mnw, nfw=nfw,
             wr=wr, cst=cst,
             wg=f(inp["exp_w_gate"])[0].reshape(NE, 8, 128, 512).transpose(0, 2, 1, 3).reshape(NE * 128, 8 * 512),
             wu=f(inp["exp_w_up"])[0].reshape(NE, 8, 128, 512).transpose(0, 2, 1, 3).reshape(NE * 128, 8 * 512),
             wd=f(inp["exp_w_down"])[0].reshape(NE, 4, 128, D).transpose(0, 2, 1, 3).reshape(NE * 128, 4 * D))
    return {k: np.ascontiguousarray(v, dtype=np.float32) for k, v in d.items()}


def kernel(**inputs):
    x = np.asarray(inputs["x"], dtype=np.float32)
    B, L, _ = x.shape
    ncores = 8
    nseq = B // ncores
    shared = _shared_inputs(inputs)
    K = build(nseq, L)
    in_maps = []
    for c in range(ncores):
        m = dict(shared)
        m["x"] = np.ascontiguousarray(x[c * nseq:(c + 1) * nseq].reshape(nseq * L, D))
        in_maps.append(m)
    res = run_bass_kernel_spmd(K.nc, in_maps, core_ids=list(range(ncores)))
    outs = [np.asarray(r["out"], dtype=np.float32).reshape(nseq, L, D) for r in res.results]
    return np.concatenate(outs, axis=0)
```
